# Optimizing a Trainium2 kernel written in Bass

```python
import jax, jax.numpy as jnp
from jax import lax
import numpy as np

D_MODEL = 2048
BATCH = 8
SEQ = 2048
DEPTH = 2

MEM_LEN = 256
N_GROUPS = 4
GROUP_WIDTH = D_MODEL // N_GROUPS
HEAD_DIM = 128
N_HEADS = GROUP_WIDTH // HEAD_DIM
ATTN_Q_BLOCK = 128
MLA_Q_RANK = 512
MLA_KV_RANK = 256
MLA_NOPE_DIM = 128
MLA_ROPE_DIM = 64
MLA_V_DIM = 128
ROPE_THETA = 10000.0
GMLP_CHUNK = 128
GMLP_GROUPS = 4
GMLP_GROUP_CH = GROUP_WIDTH // GMLP_GROUPS
MOBA_BLOCK = 256
MOBA_TOPK = 3
MOBA_Q_CHUNK = 32
X_HEADS = 4
X_HEAD_DIM = 128
X_WIDTH = X_HEADS * X_HEAD_DIM
D_FF = -(-(8 * D_MODEL) // (3 * 256)) * 256
EPS = 1e-6
NEG = -1e30

IN_SPLITS = (GROUP_WIDTH, GROUP_WIDTH, GROUP_WIDTH, N_HEADS,
             MLA_Q_RANK, MLA_KV_RANK, MLA_ROPE_DIM,
             GROUP_WIDTH, GROUP_WIDTH,
             GROUP_WIDTH, GROUP_WIDTH, GROUP_WIDTH)
IN_COLS = sum(IN_SPLITS)

kernel_name = 'hybrid_parallel_heads_fox_mla_gmlp_moba'


def rmsnorm(x, g):
    xf = x.astype(jnp.float32)
    y = xf * lax.rsqrt(jnp.mean(xf * xf, axis=-1, keepdims=True) + EPS)
    return (y * g.astype(jnp.float32)).astype(x.dtype)


def layernorm(x, g, b):
    xf = x.astype(jnp.float32)
    mu = jnp.mean(xf, axis=-1, keepdims=True)
    var = jnp.mean(jnp.square(xf - mu), axis=-1, keepdims=True)
    y = (xf - mu) * lax.rsqrt(var + EPS)
    return (y * g.astype(jnp.float32) + b.astype(jnp.float32)).astype(x.dtype)


def split_heads(t, h):
    b, s, _ = t.shape
    return t.reshape(b, s, h, -1).transpose(0, 2, 1, 3)


def merge_heads(t):
    b, h, s, d = t.shape
    return t.transpose(0, 2, 1, 3).reshape(b, s, h * d)


def causal_block_attention(q, k, v, decay=None):
    b, h, s, dq = q.shape
    nblk = s // ATTN_Q_BLOCK
    scale = dq ** -0.5
    kpos = jnp.arange(s)
    qb = q.reshape(b, h, nblk, ATTN_Q_BLOCK, dq).transpose(2, 0, 1, 3, 4)
    xs = (jnp.arange(nblk), qb)
    if decay is not None:
        xs = xs + (decay.reshape(b, h, nblk, ATTN_Q_BLOCK).transpose(2, 0, 1, 3),)

    def step(args):
        i, q_i = args[0], args[1]
        sc = jnp.einsum('bhqd,bhkd->bhqk', q_i, k).astype(jnp.float32) * scale
        if decay is not None:
            sc = sc + args[2][..., :, None] - decay[:, :, None, :]
        qpos = i * ATTN_Q_BLOCK + jnp.arange(ATTN_Q_BLOCK)
        sc = jnp.where(kpos[None, :] <= qpos[:, None], sc, NEG)
        p = jax.nn.softmax(sc, axis=-1).astype(v.dtype)
        return jnp.einsum('bhqk,bhkd->bhqd', p, v)

    o = lax.map(step, xs)
    return o.transpose(1, 2, 0, 3, 4).reshape(b, h, s, v.shape[-1])


def fox_mixer(q, k, v, f_logit, b_f, q_g, k_g):
    q = rmsnorm(split_heads(q, N_HEADS), q_g)
    k = rmsnorm(split_heads(k, N_HEADS), k_g)
    v = split_heads(v, N_HEADS)
    log_f = jax.nn.log_sigmoid(f_logit.astype(jnp.float32) + b_f.astype(jnp.float32))
    decay = jnp.cumsum(log_f, axis=1).transpose(0, 2, 1)
    return merge_heads(causal_block_attention(q, k, v, decay))


def apply_rope(x, cos, sin):
    half = x.shape[-1] // 2
    x1, x2 = x[..., :half], x[..., half:]
    return jnp.concatenate([x1 * cos - x2 * sin, x1 * sin + x2 * cos], axis=-1).astype(x.dtype)


def mla_mixer(c_q, c_kv, k_rope, q_lora_g, w_uq, kv_lora_g, w_ukv, q_g, k_g):
    b, s, _ = c_q.shape
    q = split_heads(rmsnorm(c_q, q_lora_g) @ w_uq, N_HEADS)
    kv = split_heads(rmsnorm(c_kv, kv_lora_g) @ w_ukv, N_HEADS)
    q_nope = rmsnorm(q[..., :MLA_NOPE_DIM], q_g[:MLA_NOPE_DIM])
    q_pe = rmsnorm(q[..., MLA_NOPE_DIM:], q_g[MLA_NOPE_DIM:])
    k_nope = rmsnorm(kv[..., :MLA_NOPE_DIM], k_g[:MLA_NOPE_DIM])
    v = kv[..., MLA_NOPE_DIM:]
    k_pe = rmsnorm(k_rope, k_g[MLA_NOPE_DIM:])[:, None]
    pos = jnp.arange(s, dtype=jnp.float32)
    inv_freq = ROPE_THETA ** (-jnp.arange(0, MLA_ROPE_DIM, 2, dtype=jnp.float32) / MLA_ROPE_DIM)
    ang = pos[:, None] * inv_freq[None, :]
    cos, sin = jnp.cos(ang), jnp.sin(ang)
    q_pe = apply_rope(q_pe, cos, sin)
    k_pe = apply_rope(k_pe, cos, sin)
    q = jnp.concatenate([q_nope, q_pe], axis=-1)
    k = jnp.concatenate([k_nope, jnp.broadcast_to(k_pe, (b, N_HEADS, s, MLA_ROPE_DIM))], axis=-1)
    return merge_heads(causal_block_attention(q, k, v))


def gmlp_mixer(u, v, ln_g, ln_b, w_s, b_s):
    b, s, _ = v.shape
    u = jax.nn.gelu(u)
    v = layernorm(jax.nn.gelu(v), ln_g, ln_b)
    vc = v.reshape(b, s // GMLP_CHUNK, GMLP_CHUNK, GMLP_GROUPS, GMLP_GROUP_CH)
    w_causal = w_s * jnp.tril(jnp.ones((GMLP_CHUNK, GMLP_CHUNK), w_s.dtype))
    mixed = jnp.einsum('gts,bnsgc->bntgc', w_causal, vc) + b_s.T[None, None, :, :, None]
    return u * mixed.reshape(b, s, GROUP_WIDTH)


def moba_mixer(q, k, v, q_g, k_g):
    b, s, _ = q.shape
    q = rmsnorm(split_heads(q, N_HEADS), q_g)
    k = rmsnorm(split_heads(k, N_HEADS), k_g)
    v = split_heads(v, N_HEADS)
    nb = -(-s // MOBA_BLOCK)
    s_pad = nb * MOBA_BLOCK
    pad = ((0, 0), (0, 0), (0, s_pad - s), (0, 0))
    q, k, v = jnp.pad(q, pad), jnp.pad(k, pad), jnp.pad(v, pad)
    kb = k.reshape(b, N_HEADS, nb, MOBA_BLOCK, HEAD_DIM)
    vb = v.reshape(b, N_HEADS, nb, MOBA_BLOCK, HEAD_DIM)
    k_mean = jnp.mean(kb, axis=3)
    gate = jnp.einsum('bhsd,bhnd->bhsn', q, k_mean).astype(jnp.float32)
    q_block = jnp.arange(s_pad) // MOBA_BLOCK
    gate = jnp.where(jnp.arange(nb)[None, :] < q_block[:, None], gate, NEG)
    top = min(MOBA_TOPK, nb)
    _, sel = lax.top_k(gate, top)
    sel_ok = jnp.arange(top)[None, :] < q_block[:, None]
    nqc = s_pad // MOBA_Q_CHUNK
    scale = HEAD_DIM ** -0.5
    gather = jax.vmap(jax.vmap(lambda blocks, idx: blocks[idx]))

    def step(args):
        i, q_i, sel_i, ok_i = args
        k_sel = gather(kb, sel_i)
        v_sel = gather(vb, sel_i)
        s_sel = jnp.einsum('bhqd,bhqjkd->bhqjk', q_i, k_sel).astype(jnp.float32) * scale
        s_sel = jnp.where(ok_i[None, None, :, :, None], s_sel, NEG)
        blk = (i * MOBA_Q_CHUNK) // MOBA_BLOCK
        k_own = lax.dynamic_index_in_dim(kb, blk, axis=2, keepdims=False)
        v_own = lax.dynamic_index_in_dim(vb, blk, axis=2, keepdims=False)
        s_own = jnp.einsum('bhqd,bhkd->bhqk', q_i, k_own).astype(jnp.float32) * scale
        qpos = i * MOBA_Q_CHUNK + jnp.arange(MOBA_Q_CHUNK)
        kpos = blk * MOBA_BLOCK + jnp.arange(MOBA_BLOCK)
        s_own = jnp.where(kpos[None, :] <= qpos[:, None], s_own, NEG)
        s_all = jnp.concatenate([s_sel.reshape(b, N_HEADS, MOBA_Q_CHUNK, top * MOBA_BLOCK), s_own], axis=-1)
        p = jax.nn.softmax(s_all, axis=-1).astype(v.dtype)
        p_sel = p[..., :top * MOBA_BLOCK].reshape(b, N_HEADS, MOBA_Q_CHUNK, top, MOBA_BLOCK)
        p_own = p[..., top * MOBA_BLOCK:]
        return (jnp.einsum('bhqjk,bhqjkd->bhqd', p_sel, v_sel)
                + jnp.einsum('bhqk,bhkd->bhqd', p_own, v_own))

    xs = (jnp.arange(nqc),
          q.reshape(b, N_HEADS, nqc, MOBA_Q_CHUNK, HEAD_DIM).transpose(2, 0, 1, 3, 4),
          sel.reshape(b, N_HEADS, nqc, MOBA_Q_CHUNK, top).transpose(2, 0, 1, 3, 4),
          sel_ok.reshape(nqc, MOBA_Q_CHUNK, top))
    o = lax.map(step, xs)
    o = o.transpose(1, 2, 0, 3, 4).reshape(b, N_HEADS, s_pad, HEAD_DIM)[:, :, :s]
    return merge_heads(o)


def memory_cross_attention(h, mem, mem_g, w_xq, w_xkv, q_g, k_g, w_xo):
    q = rmsnorm(split_heads(h @ w_xq, X_HEADS), q_g)
    kv = rmsnorm(mem, mem_g) @ w_xkv
    k = rmsnorm(split_heads(kv[..., :X_WIDTH], X_HEADS), k_g)
    v = split_heads(kv[..., X_WIDTH:], X_HEADS)
    sc = jnp.einsum('bhqd,bhmd->bhqm', q, k).astype(jnp.float32) * (X_HEAD_DIM ** -0.5)
    p = jax.nn.softmax(sc, axis=-1).astype(v.dtype)
    return merge_heads(jnp.einsum('bhqm,bhmd->bhqd', p, v)) @ w_xo


def swiglu(h, w_gate_up, w_down):
    gu = h @ w_gate_up
    return (jax.nn.silu(gu[..., :D_FF]) * gu[..., D_FF:]) @ w_down


def setup_inputs(seed: int = 0) -> dict:
    key = jax.random.key(seed)
    ks = iter(jax.random.split(key, 40))
    L = DEPTH
    f32 = jnp.float32

    def w(shape, fan_in):
        return jax.random.normal(next(ks), shape, f32) * fan_in ** -0.5

    def gain(shape):
        return 1.0 + 0.02 * jax.random.normal(next(ks), shape, f32)

    def small(shape):
        return 0.02 * jax.random.normal(next(ks), shape, f32)

    mla_qk = MLA_NOPE_DIM + MLA_ROPE_DIM
    return {
        'x': jax.random.normal(next(ks), (BATCH, SEQ, D_MODEL), f32),
        'mem': jax.random.normal(next(ks), (BATCH, MEM_LEN, D_MODEL), f32),
        'mix_norm': gain((L, D_MODEL)),
        'w_in': w((L, D_MODEL, IN_COLS), D_MODEL),
        'fox_b_f': small((L, N_HEADS)),
        'fox_q_norm': gain((L, HEAD_DIM)),
        'fox_k_norm': gain((L, HEAD_DIM)),
        'mla_q_lora_norm': gain((L, MLA_Q_RANK)),
        'mla_w_uq': w((L, MLA_Q_RANK, N_HEADS * mla_qk), MLA_Q_RANK),
        'mla_kv_lora_norm': gain((L, MLA_KV_RANK)),
        'mla_w_ukv': w((L, MLA_KV_RANK, N_HEADS * (MLA_NOPE_DIM + MLA_V_DIM)), MLA_KV_RANK),
        'mla_q_norm': gain((L, mla_qk)),
        'mla_k_norm': gain((L, mla_qk)),
        'gmlp_ln_g': gain((L, GROUP_WIDTH)),
        'gmlp_ln_b': small((L, GROUP_WIDTH)),
        'gmlp_w_s': w((L, GMLP_GROUPS, GMLP_CHUNK, GMLP_CHUNK), GMLP_CHUNK),
        'gmlp_b_s': gain((L, GMLP_GROUPS, GMLP_CHUNK)),
        'moba_q_norm': gain((L, HEAD_DIM)),
        'moba_k_norm': gain((L, HEAD_DIM)),
        'group_norm': gain((L, N_GROUPS, GROUP_WIDTH)),
        'w_out': w((L, N_GROUPS * GROUP_WIDTH, D_MODEL), N_GROUPS * GROUP_WIDTH),
        'xattn_norm': gain((L, D_MODEL)),
        'mem_norm': gain((L, D_MODEL)),
        'w_xq': w((L, D_MODEL, X_WIDTH), D_MODEL),
        'w_xkv': w((L, D_MODEL, 2 * X_WIDTH), D_MODEL),
        'xattn_q_norm': gain((L, X_HEAD_DIM)),
        'xattn_k_norm': gain((L, X_HEAD_DIM)),
        'w_xo': w((L, X_WIDTH, D_MODEL), X_WIDTH),
        'ffn_norm': gain((L, D_MODEL)),
        'w_gate_up': w((L, D_MODEL, 2 * D_FF), D_MODEL),
        'w_down': w((L, D_FF, D_MODEL), D_FF),
    }


def reference(x, mem, mix_norm, w_in, fox_b_f, fox_q_norm, fox_k_norm,
              mla_q_lora_norm, mla_w_uq, mla_kv_lora_norm, mla_w_ukv, mla_q_norm, mla_k_norm,
              gmlp_ln_g, gmlp_ln_b, gmlp_w_s, gmlp_b_s, moba_q_norm, moba_k_norm,
              group_norm, w_out, xattn_norm, mem_norm, w_xq, w_xkv, xattn_q_norm, xattn_k_norm,
              w_xo, ffn_norm, w_gate_up, w_down):
    split_idx = np.cumsum(IN_SPLITS)[:-1].tolist()
    for l in range(DEPTH):
        proj = rmsnorm(x, mix_norm[l]) @ w_in[l]
        (fq, fk, fv, ff, mcq, mckv, mkr, gu, gv, bq, bk, bv) = jnp.split(proj, split_idx, axis=-1)
        o_a = fox_mixer(fq, fk, fv, ff, fox_b_f[l], fox_q_norm[l], fox_k_norm[l])
        o_b = mla_mixer(mcq, mckv, mkr, mla_q_lora_norm[l], mla_w_uq[l], mla_kv_lora_norm[l],
                        mla_w_ukv[l], mla_q_norm[l], mla_k_norm[l])
        o_c = gmlp_mixer(gu, gv, gmlp_ln_g[l], gmlp_ln_b[l], gmlp_w_s[l], gmlp_b_s[l])
        o_d = moba_mixer(bq, bk, bv, moba_q_norm[l], moba_k_norm[l])
        mixed = jnp.concatenate([rmsnorm(o_a, group_norm[l, 0]), rmsnorm(o_b, group_norm[l, 1]),
                                 rmsnorm(o_c, group_norm[l, 2]), rmsnorm(o_d, group_norm[l, 3])], axis=-1)
        x = x + mixed @ w_out[l]
        x = x + memory_cross_attention(rmsnorm(x, xattn_norm[l]), mem, mem_norm[l], w_xq[l], w_xkv[l],
                                       xattn_q_norm[l], xattn_k_norm[l], w_xo[l])
        x = x + swiglu(rmsnorm(x, ffn_norm[l]), w_gate_up[l], w_down[l])
    return x
```

```python
import numpy as np
from contextlib import ExitStack
import concourse.bass as bass
import concourse.mybir as mybir
from concourse.bass_utils import run_bass_kernel_spmd

F32 = mybir.dt.float32
BF16 = mybir.dt.bfloat16
AF = mybir.ActivationFunctionType
ALU = mybir.AluOpType
AX = mybir.AxisListType

COMPUTE = ('pe', 'act', 'dve', 'pool')
NDMASEM = 8
SAME_ENGINE_SYNC = True


class Slot:
    __slots__ = ('w', 'r', 'name')

    def __init__(self, name=''):
        self.w = None
        self.r = {}
        self.name = name


class Op:
    __slots__ = ('eng', 'fn', 'deps', 'sig', 'val', 'sem', 'isdma', 'key')

    def __init__(self, eng, fn):
        self.eng = eng
        self.fn = fn
        self.deps = set()
        self.sig = False
        self.val = 0
        self.sem = None
        self.isdma = False
        self.key = eng


class Prog:
    def __init__(self, nc):
        self.nc = nc
        self.ops = {e: [] for e in COMPUTE + ('sp',)}
        self.dma_count = {'sp': 0, 'pool': 0, 'act': 0}
        self.dma_last = {}
        self.nops = 0
        self.pending = {}
        self.last_compute = {}

    def slot(self, name=''):
        return Slot(name)

    def slots(self, n, name=''):
        return [Slot(name + str(i)) for i in range(n)]

    def barrier(self):
        last = set(self.last_compute.values()) | set(self.dma_last.values())
        self.pending = {e: set(last) for e in self.ops}

    def _add(self, o, reads, writes):
        deps = o.deps
        pend = self.pending.pop(o.eng, None)
        if pend:
            deps |= pend
        if not o.isdma:
            self.last_compute[o.eng] = o
        for s in reads:
            if s.w is not None:
                deps.add(s.w)
        for s in writes:
            if s.w is not None:
                deps.add(s.w)
            for r in s.r.values():
                deps.add(r)
        for s in writes:
            s.w = o
            s.r = {}
        for s in reads:
            s.r[o.key] = o
        deps.discard(o)
        if o.eng == 'pe' and not o.isdma:
            o.deps = {d for d in deps if d.isdma or d.eng != 'pe'}
        elif not SAME_ENGINE_SYNC and not o.isdma:
            o.deps = {d for d in deps if d.isdma or d.eng != o.eng}
        self.ops[o.eng].append(o)
        self.nops += 1
        return o

    def op(self, eng, fn, reads=(), writes=()):
        return self._add(Op(eng, fn), reads, writes)

    def dma(self, eng, fn, reads=(), writes=()):
        o = Op(eng, fn)
        o.isdma = True
        k = self.dma_count[eng]
        self.dma_count[eng] = k + 1
        o.key = (eng, k % NDMASEM)
        o.sem = o.key
        o.val = 16 * (k // NDMASEM + 1)
        prev = self.dma_last.get(o.key)
        if prev is not None:
            o.deps.add(prev)
        self.dma_last[o.key] = o
        return self._add(o, reads, writes)

    def emit(self):
        nc = self.nc
        for e in self.ops:
            for o in self.ops[e]:
                for d in o.deps:
                    if not d.isdma:
                        d.sig = True
        with ExitStack() as es:
            sems = {}
            for e in COMPUTE:
                sems[e] = es.enter_context(nc.semaphore('s_' + e))
            for q in self.dma_count:
                for i in range(NDMASEM):
                    sems[(q, i)] = es.enter_context(nc.semaphore('d_%s%d' % (q, i)))
            for e in COMPUTE:
                c = 0
                for o in self.ops[e]:
                    if o.isdma:
                        continue
                    if o.sig:
                        c += 1
                        o.val = c
                        o.sem = e
            block = es.enter_context(nc.Block())
            ops = self.ops
            dma_last = self.dma_last

            def run(e, engine):
                seen = {}
                for o in ops[e]:
                    for d in o.deps:
                        if seen.get(d.sem, 0) < d.val:
                            engine.wait_ge(sems[d.sem], d.val)
                            seen[d.sem] = d.val
                    ins = o.fn(engine)
                    if o.isdma:
                        ins.then_inc(sems[o.sem], 16)
                    elif o.sig:
                        ins.then_inc(sems[e], 1)
                if e == 'sp':
                    for key, o in dma_last.items():
                        if seen.get(o.sem, 0) < o.val:
                            engine.wait_ge(sems[o.sem], o.val)

            @block.tensor
            def _(eng):
                run('pe', eng)

            @block.scalar
            def _(eng):
                run('act', eng)

            @block.vector
            def _(eng):
                run('dve', eng)

            @block.gpsimd
            def _(eng):
                run('pool', eng)

            @block.sync
            def _(eng):
                run('sp', eng)


S = 2048
DM = 2048
DFF = 5632
EPS = 1e-6
OFF = dict(fq=0, fk=512, fv=1024, ff=1536, mcq=1540, mckv=2052, mkr=2308, gu=2372, gv=2884,
           bq=3396, bk=3908, bv=4420)
WSH = [('mix_norm', (2, 2048)), ('w_in', (2, 2048, 4932)), ('fox_b_f', (2, 4)), ('fox_q_norm', (2, 128)),
       ('fox_k_norm', (2, 128)), ('mla_q_lora_norm', (2, 512)), ('mla_w_uq', (2, 512, 768)),
       ('mla_kv_lora_norm', (2, 256)), ('mla_w_ukv', (2, 256, 1024)), ('mla_q_norm', (2, 192)),
       ('mla_k_norm', (2, 192)), ('gmlp_ln_g', (2, 512)), ('gmlp_ln_b', (2, 512)),
       ('gmlp_w_s', (2, 4, 128, 128)), ('gmlp_b_s', (2, 4, 128)), ('moba_q_norm', (2, 128)),
       ('moba_k_norm', (2, 128)), ('group_norm', (2, 4, 512)), ('w_out', (2, 2048, 2048)),
       ('xattn_norm', (2, 2048)), ('mem_norm', (2, 2048)), ('w_xq', (2, 2048, 512)),
       ('w_xkv', (2, 2048, 1024)), ('xattn_q_norm', (2, 128)), ('xattn_k_norm', (2, 128)),
       ('w_xo', (2, 512, 2048)), ('ffn_norm', (2, 2048)), ('w_gate_up', (2, 2048, 11264)),
       ('w_down', (2, 5632, 2048))]
C32W = 128 * 4 + 64 + 512 + 128
CBW = 128 + 128 + 1024
NEGBIG = -30000.0


def host_consts():
    import ml_dtypes
    c32 = np.zeros((128, C32W), np.float32)
    c32[:, 0:128] = np.eye(128)
    k = np.arange(128)[:, None]
    q = np.arange(128)[None, :]
    c32[:, 128:256] = np.where(q >= k, 0.0, NEGBIG)
    c32[:, 256:384] = (q <= k).astype(np.float32)
    c32[:, 384:512] = 1.0
    rot = np.zeros((64, 64), np.float32)
    for i in range(32):
        rot[i + 32, i] = -1.0
        rot[i, i + 32] = 1.0
    c32[0:64, 512:576] = rot
    for h in range(4):
        c32[h, 576 + h * 128:576 + (h + 1) * 128] = -np.sqrt(128.0)
    mneg = np.zeros((8, 8), np.float32)
    for qb in range(8):
        for n in range(8):
            mneg[qb, n] = 0.0 if n < qb else (1e30 if n == qb else -1e30)
    c32[:, 1088:1216] = np.concatenate([mneg[t // 2] for t in range(16)]).reshape(1, 128)
    cb = np.zeros((128, CBW), np.float32)
    cb[:, 0:128] = np.eye(128)
    cb[:, 128:256] = 1.0
    for n in range(8):
        cb[n, 256 + n * 128:256 + (n + 1) * 128] = 1.0
    pos = np.arange(S, dtype=np.float32)
    inv = (np.float32(10000.0) ** (-np.arange(0, 64, 2, dtype=np.float32) / np.float32(64))).astype(np.float32)
    ang = (pos[:, None] * inv[None, :]).astype(np.float32)
    cos = np.cos(ang).astype(np.float32).T
    sin = np.sin(ang).astype(np.float32).T
    cs = np.concatenate([cos, cos, sin, sin], 0).astype(np.float32)
    return dict(c32=c32, cb=cb.astype(ml_dtypes.bfloat16), cossin=np.ascontiguousarray(cs))


class Rot:
    def __init__(self, items):
        self.items = items
        self.i = 0

    def next(self):
        it = self.items[self.i % len(self.items)]
        self.i += 1
        return it


def build(nc, L=2, phases=None, dbg=()):
    P = Prog(nc)
    es = ExitStack()
    D = {}
    for n, s in [('x', (S, DM)), ('mem', (256, DM))] + WSH:
        D[n] = nc.dram_tensor(n, list(s), F32, kind="ExternalInput").ap()
    c32_d = nc.dram_tensor("c32", [128, C32W], F32, kind="ExternalInput").ap()
    cb_d = nc.dram_tensor("cb", [128, CBW], BF16, kind="ExternalInput").ap()
    cs_d = nc.dram_tensor("cossin", [128, S], F32, kind="ExternalInput").ap()
    y_d = nc.dram_tensor("y", [S, DM], F32, kind="ExternalOutput").ap()
    mixT_d = nc.dram_tensor("mixT", [DM, S], BF16).ap()
    dbg_d = {}
    for name, shape, dt in dbg:
        dbg_d[name] = nc.dram_tensor(name, list(shape), dt, kind="ExternalOutput").ap()

    ARENA = 211200
    arena = es.enter_context(nc.sbuf_tensor("arena", [128, ARENA // 2], BF16))
    ps = [es.enter_context(nc.psum_tensor("ps%d" % i, [128, 512], F32)) for i in range(8)]
    S_ps = P.slots(8, 'ps')
    psA = Rot([(ps[i][:], S_ps[i]) for i in range(4)])
    psB = Rot([(ps[i][:], S_ps[i]) for i in range(4, 8)])

    st = {'off': 0}

    def alloc(shape, dt, name=''):
        n = 1
        for d in shape[1:]:
            n *= d
        nb = n * (4 if dt == F32 else 2)
        nb = (nb + 63) // 64 * 64
        o = st['off']
        assert o + nb <= ARENA, (name, o, nb)
        st['off'] = o + nb
        a = arena[:, o // 2:(o + nb) // 2]
        if dt == F32:
            a = a.bitcast(F32)
        a = a[:, 0:n]
        if len(shape) == 3:
            a = a.rearrange("p (a b) -> p a b", a=shape[1])
        if shape[0] < 128:
            a = a[0:shape[0]]
        return a

    actT = alloc([128, 16, S], BF16, 'actT')
    S_act = P.slots(16, 'act')
    cb = alloc([128, CBW], BF16)
    c32 = alloc([128, C32W], F32)
    S_c = P.slot('consts')
    identb = cb[:, 0:128]
    onesb = cb[:, 128:256]
    onehot8 = cb[0:8, 256:1280]
    ident32 = c32[:, 0:128]
    maskT = c32[:, 128:256]
    tril = c32[:, 256:384]
    ones32 = c32[:, 384:512]
    rotT = c32[0:64, 512:576]
    selneg = c32[0:4, 576:1088]
    mneg16 = c32[:, 1088:1216]
    gcol = alloc([128, 40], F32)
    S_gcol = P.slot('gcol')
    ssq = alloc([128, 16], F32)
    rsq = alloc([128, 16], F32)
    S_ssq = P.slot()
    S_rsq = P.slot()
    PH0 = st['off']
    S_y = [P.slots(4, 'y%d_' % t) for t in range(16)]
    S_mix = [P.slots(4, 'mix%d_' % g) for g in range(4)]

    P.dma('sp', lambda e: e.dma_start(out=cb, in_=cb_d), writes=[S_c])
    P.dma('sp', lambda e: e.dma_start(out=c32, in_=c32_d), writes=[S_c])

    def phase():
        P.barrier()
        st['off'] = PH0

    def col_load(vec, c):
        n = vec.shape[0]
        P.dma('sp', lambda e: e.dma_start(out=gcol[0:n, c:c + 1], in_=vec.rearrange("(p o) -> p o", o=1)),
              writes=[S_gcol])

    def norm_T(src, S_src, gvec, dstT, S_dst, ntile, copy=None, S_copy=None):
        xs = [alloc([128, DM], F32) for _ in range(2)]
        S_xs = P.slots(2)
        xnb = [alloc([128, DM], BF16) for _ in range(2)]
        S_xnb = P.slots(2)
        junk = alloc([128, DM], BF16)
        S_junk = P.slot()
        gbc = alloc([128, DM], F32)
        S_gbc = P.slot('gbc')
        P.dma('sp', lambda e: e.dma_start(out=gbc, in_=gvec.partition_broadcast(128)), writes=[S_gbc])
        P.op('pool', lambda e: e.memset(ssq, 0.0), writes=[S_ssq])
        for tt in range(ntile):
            i = tt % 2
            P.dma('sp', lambda e, i=i, tt=tt: e.dma_start(out=xs[i], in_=src(tt)), reads=S_src(tt), writes=[S_xs[i]])
            if copy is not None:
                P.dma('sp', lambda e, i=i, tt=tt: e.dma_start(out=copy(tt), in_=xs[i]), reads=[S_xs[i]],
                      writes=S_copy(tt))
            P.op('act', lambda e, i=i, tt=tt: e.activation(out=junk, in_=xs[i], func=AF.Square,
                                                           accum_out=ssq[:, tt:tt + 1]),
                 reads=[S_xs[i]], writes=[S_junk, S_ssq])
            P.op('act', lambda e, tt=tt: e.activation(out=rsq[:, tt:tt + 1], in_=ssq[:, tt:tt + 1], func=AF.Sqrt,
                                                      bias=EPS, scale=1.0 / DM), reads=[S_ssq], writes=[S_rsq])
            P.op('dve', lambda e, tt=tt: e.reciprocal(out=rsq[:, tt:tt + 1], in_=rsq[:, tt:tt + 1]),
                 reads=[S_rsq], writes=[S_rsq])
            P.op('dve', lambda e, i=i, tt=tt: e.scalar_tensor_tensor(out=xnb[i], in0=xs[i], scalar=rsq[:, tt:tt + 1],
                                                                     in1=gbc, op0=ALU.mult, op1=ALU.mult),
                 reads=[S_xs[i], S_rsq, S_gbc], writes=[S_xnb[i]])
            for half in range(2):
                bank, S_b = psA.next()
                pT = bank.bitcast(BF16).rearrange("p (a b) -> p a b", a=8)
                for j in range(8):
                    kc = half * 8 + j
                    P.op('pe', lambda e, pT=pT, j=j, kc=kc, i=i: e.transpose(
                        out=pT[:, j, :], in_=xnb[i][:, kc * 128:(kc + 1) * 128], identity=identb),
                        reads=[S_xnb[i], S_c], writes=[S_b])
                eng = 'act' if half == 0 else 'dve'
                dst = dstT[:, half * 8:(half + 1) * 8, tt * 128:(tt + 1) * 128]
                if eng == 'act':
                    P.op('act', lambda e, dst=dst, pT=pT: e.activation(out=dst, in_=pT, func=AF.Copy),
                         reads=[S_b], writes=[S_dst[tt]])
                else:
                    P.op('dve', lambda e, dst=dst, pT=pT: e.tensor_copy(out=dst, in_=pT), reads=[S_b],
                         writes=[S_dst[tt]])

    wst = {}

    def wbufs_alloc(n, widths=None):
        widths = widths or [520] * n
        wst['flat'] = [alloc([128, 16 * wd], BF16) for wd in widths]
        wst['wd'] = widths
        wst['S'] = P.slots(n, 'w')
        wst['rot'] = Rot(list(range(n)))

    def load_slab(src, kch, ncols, kind='k16'):
        i = wst['rot'].next()
        flat = wst['flat'][i]
        if kind == 'k16':
            assert ncols <= wst['wd'][i]
            v = flat.rearrange("p (k n) -> p k n", k=16)
        else:
            v = flat[:, 0:8192].rearrange("p (k n) -> p k n", k=4)
        dst = v[:, 0:kch, 0:ncols]
        P.dma('pool', lambda e: e.dma_start(out=dst, in_=src.rearrange("(kc p) n -> p kc n", p=128)),
              writes=[wst['S'][i]])
        return v, wst['S'][i]

    def gemm_fm(bank, S_b, M, w, S_w, c0, tt, width=512, src=None, S_src=None, nk=16, t0=None):
        src = actT if src is None else src
        if t0 is None:
            t0 = tt * 512
        rd = [S_w] + (S_act[t0 // 128:(t0 + width + 127) // 128] if S_src is None else S_src)
        for kc in range(nk):
            P.op('pe', lambda e, kc=kc: e.matmul(bank[0:M, 0:width], lhsT=w[:, kc, c0:c0 + M],
                                                 rhs=src[:, kc, t0:t0 + width], start=(kc == 0), stop=(kc == nk - 1)),
                 reads=rd, writes=[S_b])

    def gemm_tm(bank, S_b, N, w, S_w, c0, t128, src=None, S_src=None, nk=16):
        src = actT if src is None else src
        rd = [S_w] + ([S_act[t128]] if S_src is None else S_src)
        for kc in range(nk):
            P.op('pe', lambda e, kc=kc: e.matmul(bank[:, 0:N], lhsT=src[:, kc, t128 * 128:(t128 + 1) * 128],
                                                 rhs=w[:, kc, c0:c0 + N], start=(kc == 0), stop=(kc == nk - 1)),
                 reads=rd, writes=[S_b])

    nb = {}

    def norm_bufs(ntmp=4):
        if ntmp:
            nb['tmp'] = alloc([128, ntmp, 512], F32)
        nb['S_tmp'] = P.slots(4)
        nb['sq'] = alloc([128, 4, 512], BF16)
        nb['S_sq'] = P.slot()
        nb['r32'] = alloc([128, 512], F32)
        nb['S_r32'] = P.slot()

    def rstd_from_bank(bank, S_b, M, width, nfeat, r32, S_r32):
        P.op('act', lambda e: e.activation(out=r32[0:M, 0:width], in_=bank[0:M, 0:width], func=AF.Ln, bias=EPS,
                                           scale=1.0 / nfeat), reads=[S_b], writes=[S_r32])
        P.op('act', lambda e: e.activation(out=r32[0:M, 0:width], in_=r32[0:M, 0:width], func=AF.Exp, scale=-0.5),
             reads=[S_r32], writes=[S_r32])

    def fm_norm(tmp, S_tmp, M, nch, gc0, outs, S_outs, width=512):
        sq, r32 = nb['sq'], nb['r32']
        S_sq, S_r32 = nb['S_sq'], nb['S_r32']
        for c in range(nch):
            P.op('act', lambda e, c=c: e.activation(out=sq[0:M, c, 0:width], in_=tmp[0:M, c, 0:width], func=AF.Square),
                 reads=[S_tmp[c]], writes=[S_sq])
        bank, S_b = psB.next()
        for c in range(nch):
            P.op('pe', lambda e, c=c: e.matmul(bank[0:M, 0:width], lhsT=onesb[0:M, 0:M], rhs=sq[0:M, c, 0:width],
                                               start=(c == 0), stop=(c == nch - 1)),
                 reads=[S_sq, S_c], writes=[S_b])
        rstd_from_bank(bank, S_b, M, width, M * nch, r32, S_r32)
        for c in range(nch):
            o_ap = outs(c)
            P.op('dve', lambda e, c=c, o_ap=o_ap: e.scalar_tensor_tensor(out=o_ap, in0=tmp[0:M, c, 0:width],
                                                              scalar=gcol[0:M, gc0 + c:gc0 + c + 1],
                                                              in1=r32[0:M, 0:width], op0=ALU.mult, op1=ALU.mult),
                 reads=[S_tmp[c], S_r32, S_gcol], writes=S_outs(c))

    def evac(bank, S_b, dst, S_dst, eng='act', M=128, width=512):
        if eng == 'act':
            P.op('act', lambda e: e.activation(out=dst, in_=bank[0:M, 0:width], func=AF.Copy), reads=[S_b], writes=S_dst)
        else:
            P.op('dve', lambda e: e.tensor_copy(out=dst, in_=bank[0:M, 0:width]), reads=[S_b], writes=S_dst)

    pn = {'pending': None, 'i': 0, 'bufs': None}

    def pn_bufs():
        pn['bufs'] = [(alloc([128, 512], BF16), P.slot(), alloc([128, 512], F32), P.slot()) for _ in range(2)]
        pn['pending'] = None

    def pn_flush():
        if pn['pending'] is not None:
            f = pn['pending']
            pn['pending'] = None
            f()

    def proj_norm_fm(w, S_w, c0, M, tt, gc, out, S_out, src=None, S_src=None, nk=16, t0=None, width=512, post=None):
        sq, S_sq, r32, S_r32 = pn['bufs'][pn['i'] % 2]
        pn['i'] += 1
        bank, S_b = psA.next()
        gemm_fm(bank, S_b, M, w, S_w, c0, tt, width=width, src=src, S_src=S_src, nk=nk, t0=t0)
        P.op('act', lambda e: e.activation(out=sq[0:M, 0:width], in_=bank[0:M, 0:width], func=AF.Square),
             reads=[S_b], writes=[S_sq])

        def stage2():
            b2, S_b2 = psB.next()
            P.op('pe', lambda e: e.matmul(b2[0:M, 0:width], lhsT=onesb[0:M, 0:M], rhs=sq[0:M, 0:width], start=True,
                                          stop=True), reads=[S_sq, S_c], writes=[S_b2])
            rstd_from_bank(b2, S_b2, M, width, M, r32, S_r32)
            P.op('dve', lambda e: e.scalar_tensor_tensor(out=out, in0=bank[0:M, 0:width], scalar=gcol[0:M, gc:gc + 1],
                                                         in1=r32[0:M, 0:width], op0=ALU.mult, op1=ALU.mult),
                 reads=[S_b, S_r32, S_gcol], writes=S_out)
            if post is not None:
                post()

        prev = pn['pending']
        pn['pending'] = stage2
        if prev is not None:
            prev()

    ab = {}

    def attn_bufs():
        ab['pt'] = [alloc([128, 512], BF16) for _ in range(4)]
        ab['S_pt'] = P.slots(4)
        ab['rot'] = Rot([0, 1, 2, 3])
        ab['rsum'] = alloc([128, 512], F32)
        ab['S_rsum'] = P.slot()

    def attention(qts, H, terms, v_of, scale, causal, nkt_all, oT32, S_o, dve_bias=None, act_bias=None,
                  setup=None, tail=None):
        rsum, S_rsum = ab['rsum'], ab['S_rsum']
        pairs = [(qt, h) for qt in qts for h in range(H)]
        if setup is not None:
            setup(*pairs[0])
        for pi_, (qt, h) in enumerate(pairs):
            q0 = qt * 512
            if setup is not None and pi_ + 1 < len(pairs):
                setup(*pairs[pi_ + 1])
            po, S_po = psB.next()
            psm, S_psm = psB.next()
            nkt = (4 * qt + 4) if causal else nkt_all

            def qk(kt):
                c0 = max(0, kt * 128 - q0) if causal else 0
                sb, S_sb = psA.next()
                tl = terms(h, kt, q0 + c0, q0 + 512, c0)
                for i, (l, r, rd) in enumerate(tl):
                    P.op('pe', lambda e, l=l, r=r, i=i, sb=sb, c0=c0, n=len(tl): e.matmul(
                        sb[:, c0:512], lhsT=l, rhs=r, start=(i == 0), stop=(i == n - 1)), reads=rd, writes=[S_sb])
                if causal and kt * 128 >= q0:
                    P.op('dve', lambda e, sb=sb, c0=c0: e.tensor_tensor(out=sb[:, c0:c0 + 128], in0=sb[:, c0:c0 + 128],
                                                                        in1=maskT, op=ALU.add),
                         reads=[S_sb, S_c], writes=[S_sb])
                if dve_bias is not None:
                    dve_bias(qt, h, kt, sb, S_sb, c0)
                pi = ab['rot'].next()
                pt, S_pt = ab['pt'][pi], ab['S_pt'][pi]
                bias, brd = act_bias(h, kt) if act_bias is not None else (0.0, [])
                P.op('act', lambda e, pt=pt, sb=sb, c0=c0, bias=bias: e.activation(
                    out=pt[:, c0:512], in_=sb[:, c0:512], func=AF.Exp, bias=bias, scale=scale),
                    reads=[S_sb] + brd, writes=[S_pt])
                return pt, S_pt, c0

            def pv(kt, pt, S_pt, c0):
                vap, vrd = v_of(h, kt)
                P.op('pe', lambda e, po=po, vap=vap, pt=pt, c0=c0, kt=kt, nkt=nkt: e.matmul(
                    po[:, c0:512], lhsT=vap, rhs=pt[:, c0:512], start=(kt == 0), stop=(kt == nkt - 1)),
                    reads=[S_pt] + vrd, writes=[S_po])
                P.op('pe', lambda e, psm=psm, pt=pt, c0=c0, kt=kt, nkt=nkt: e.matmul(
                    psm[:, c0:512], lhsT=onesb, rhs=pt[:, c0:512], start=(kt == 0), stop=(kt == nkt - 1)),
                    reads=[S_pt, S_c], writes=[S_psm])

            pend = []
            for kt in range(nkt):
                pend.append((kt,) + qk(kt))
                if len(pend) > 2:
                    pv(*pend.pop(0))
            while pend:
                pv(*pend.pop(0))
            P.op('act', lambda e, psm=psm: e.activation(out=rsum, in_=psm, func=AF.Ln), reads=[S_psm], writes=[S_rsum])
            P.op('act', lambda e: e.activation(out=rsum, in_=rsum, func=AF.Exp, scale=-1.0), reads=[S_rsum], writes=[S_rsum])
            P.op('dve', lambda e, po=po, h=h: e.tensor_tensor(out=oT32[:, h, :], in0=po, in1=rsum, op=ALU.mult),
                 reads=[S_po, S_rsum], writes=[S_o[h]])
            if tail is not None and h == H - 1:
                tail(qt)

    def group_tail(g, oT32, S_o, ostage, S_ost):
        def tail(qt):
            fm_norm(oT32, S_o, 128, 4, 14 + g * 4, lambda c: ostage[:, c, :], lambda c: [S_ost])
            dst = mixT_d[g * 512:(g + 1) * 512, qt * 512:(qt + 1) * 512].rearrange("(c p) t -> p c t", p=128)
            P.dma('sp', lambda e: e.dma_start(out=dst, in_=ostage), reads=[S_ost], writes=[S_mix[g][qt]])
        return tail

    def dbg_out(name, ap, S_rd, view=None):
        if name in dbg_d:
            d = dbg_d[name] if view is None else view(dbg_d[name])
            P.dma('sp', lambda e: e.dma_start(out=d, in_=ap), reads=S_rd)

    def run_if(name):
        def deco(f):
            if phases is None or name in phases:
                f()
            return f
        return deco

    def layer(l):
        w_in = D['w_in'][l]
        _layer_body(l, w_in)

    def _layer_body(l, w_in):
        phase()
        col_load(D['fox_q_norm'][l], 0)
        col_load(D['fox_k_norm'][l], 1)
        for c in range(4):
            col_load(D['mla_q_lora_norm'][l][c * 128:(c + 1) * 128], 2 + c)
        for c in range(2):
            col_load(D['mla_kv_lora_norm'][l][c * 128:(c + 1) * 128], 6 + c)
        col_load(D['mla_q_norm'][l][0:128], 8)
        col_load(D['mla_q_norm'][l][128:192], 9)
        col_load(D['mla_k_norm'][l][0:128], 10)
        col_load(D['mla_k_norm'][l][128:192], 11)
        col_load(D['moba_q_norm'][l], 12)
        col_load(D['moba_k_norm'][l], 13)
        for g in range(4):
            for c in range(4):
                col_load(D['group_norm'][l][g][c * 128:(c + 1) * 128], 14 + g * 4 + c)
        col_load(D['xattn_q_norm'][l], 30)
        col_load(D['xattn_k_norm'][l], 31)
        col_load(D['fox_b_f'][l], 32)
        P.op('dve', lambda e: e.tensor_scalar(out=gcol[0:4, 32:33], in0=gcol[0:4, 32:33], scalar1=-1.0, scalar2=0.0,
                                              op0=ALU.mult, op1=ALU.add), reads=[S_gcol], writes=[S_gcol])

        if l == 0:
            norm_T(lambda tt: D['x'][tt * 128:(tt + 1) * 128, :], lambda tt: [], D['mix_norm'][l], actT, S_act, 16,
                   copy=lambda tt: y_d[tt * 128:(tt + 1) * 128, :], S_copy=lambda tt: S_y[tt])
        else:
            norm_T(lambda tt: y_d[tt * 128:(tt + 1) * 128, :], lambda tt: S_y[tt], D['mix_norm'][l], actT, S_act, 16)
        if l == 0:
            dbg_out('d_xnT', actT, S_act)

        def qk_bufs():
            b = {}
            b['qT'] = alloc([128, 4, S], BF16)
            b['S_q'] = [P.slots(4) for _ in range(4)]
            b['kT'] = alloc([128, 4, S], BF16)
            b['S_k'] = [P.slots(4) for _ in range(4)]
            b['v'] = alloc([128, 16, 512], BF16)
            b['S_v'] = P.slots(16)
            b['oT32'] = alloc([128, 4, 512], F32)
            b['S_o'] = P.slots(4)
            b['ostage'] = alloc([128, 4, 512], BF16)
            b['S_ost'] = P.slot()
            return b

        def v_proj(b, wv, S_wv, c0):
            for t in range(16):
                bank, S_b = psA.next()
                gemm_tm(bank, S_b, 512, wv, S_wv, c0, t)
                evac(bank, S_b, b['v'][:, t, :], [b['S_v'][t]], eng='act' if t % 2 == 0 else 'dve')

        def qk_proj(b, key, Sk, w, S_w, gc):
            for tt in range(4):
                for h in range(4):
                    proj_norm_fm(w, S_w, h * 128, 128, tt, gc, b[key][:, h, tt * 512:(tt + 1) * 512], [b[Sk][h][tt]])
            pn_flush()

        @run_if('A')
        def ph_A():
            phase()
            wbufs_alloc(2)
            norm_bufs(0)
            pn_bufs()
            attn_bufs()
            b = qk_bufs()
            sp32 = alloc([4, 512], F32)
            cs = alloc([4, S], F32)
            S_sp = P.slot()
            S_cs = P.slot()
            ones4 = alloc([4, 512], F32)
            S_ones4 = P.slot()
            ckey = alloc([128, 64], F32)
            S_ckey = P.slot()
            dqbcs = [alloc([128, 512], F32) for _ in range(2)]
            S_dqs = P.slots(2)
            dqmap = {}
            etmp = alloc([4, 512], F32)
            S_et = P.slot()
            P.op('pool', lambda e: e.memset(ones4, 1.0), writes=[S_ones4])
            wq, S_wq = load_slab(w_in[:, OFF['fq']:OFF['fq'] + 512], 16, 512)
            wk, S_wk = load_slab(w_in[:, OFF['fk']:OFF['fk'] + 512], 16, 512)
            qk_proj(b, 'qT', 'S_q', wq, S_wq, 0)
            wv, S_wv = load_slab(w_in[:, OFF['fv']:OFF['fv'] + 516], 16, 516)
            qk_proj(b, 'kT', 'S_k', wk, S_wk, 1)
            v_proj(b, wv, S_wv, 0)
            for tt in range(4):
                bank, S_b = psA.next()
                gemm_fm(bank, S_b, 4, wv, S_wv, 512, tt)
                P.op('act', lambda e, bank=bank: e.activation(out=etmp, in_=bank[0:4, :], func=AF.Exp,
                                                              bias=gcol[0:4, 32:33], scale=-1.0),
                     reads=[S_b, S_gcol], writes=[S_et])
                P.op('act', lambda e, tt=tt: e.activation(out=sp32, in_=etmp, func=AF.Ln,
                                                          bias=1.0, scale=1.0), reads=[S_et], writes=[S_sp])
                init = 0.0 if tt == 0 else cs[:, tt * 512 - 1:tt * 512]
                P.op('dve', lambda e, tt=tt, init=init: e.tensor_tensor_scan(
                    out=cs[:, tt * 512:(tt + 1) * 512], data0=ones4, data1=sp32,
                    initial=init, op0=ALU.mult, op1=ALU.add), reads=[S_sp, S_ones4, S_cs], writes=[S_cs])
            bank, S_b = psA.next()
            for t in range(16):
                P.op('pe', lambda e, t=t, bank=bank: e.transpose(out=bank[:, t * 4:(t + 1) * 4],
                                                                 in_=cs[:, t * 128:(t + 1) * 128],
                                                                 identity=ident32[0:4, 0:4]),
                     reads=[S_cs, S_c], writes=[S_b])
            P.op('dve', lambda e, bank=bank: e.tensor_copy(out=ckey, in_=bank[:, 0:64]), reads=[S_b], writes=[S_ckey])
            dbg_out('d_cs', cs, [S_cs])
            dbg_out('d_fq', b['qT'], [s for hh in b['S_q'] for s in hh])

            def fox_setup(qt, h):
                di = len(dqmap) % 2
                dqmap[(qt, h)] = di
                dqbc, S_dq = dqbcs[di], S_dqs[di]
                bank, S_b = psB.next()
                P.op('pe', lambda e: e.matmul(bank, lhsT=selneg[:, h * 128:(h + 1) * 128],
                                              rhs=cs[:, qt * 512:(qt + 1) * 512], start=True, stop=True),
                     reads=[S_cs, S_c], writes=[S_b])
                P.op('act', lambda e: e.activation(out=dqbc, in_=bank, func=AF.Copy), reads=[S_b], writes=[S_dq])

            def fox_dve_bias(qt, h, kt, sb, S_sb, c0):
                di = dqmap[(qt, h)]
                dqbc, S_dq = dqbcs[di], S_dqs[di]
                P.op('dve', lambda e: e.tensor_tensor(out=sb[:, c0:512], in0=sb[:, c0:512], in1=dqbc[:, c0:512],
                                                      op=ALU.add), reads=[S_sb, S_dq], writes=[S_sb])

            attention(range(4), 4,
                      lambda h, kt, qa, qb_, c0: [(b['kT'][:, h, kt * 128:(kt + 1) * 128], b['qT'][:, h, qa:qb_],
                                                  [b['S_k'][h][kt // 4], b['S_q'][h][qa // 512]])],
                      lambda h, kt: (b['v'][:, kt, h * 128:(h + 1) * 128], [b['S_v'][kt]]),
                      128 ** -0.5, True, 16, b['oT32'], b['S_o'],
                      dve_bias=fox_dve_bias,
                      act_bias=lambda h, kt: (ckey[:, kt * 4 + h:kt * 4 + h + 1], [S_ckey]),
                      setup=fox_setup, tail=group_tail(0, b['oT32'], b['S_o'], b['ostage'], b['S_ost']))

        @run_if('B')
        def ph_B():
            phase()
            b = {}
            b['qT'] = alloc([128, 4, S], BF16)
            b['S_q'] = [P.slots(4) for _ in range(4)]
            b['kT'] = alloc([128, 4, S], BF16)
            b['S_k'] = [P.slots(4) for _ in range(4)]
            b['v'] = alloc([128, 16, 512], BF16)
            b['S_v'] = P.slots(16)
            qpe = alloc([64, 4, S], BF16)
            S_qpe = [P.slots(4) for _ in range(4)]
            kpe = alloc([64, S], BF16)
            S_kpe = P.slots(4)
            keepB = st['off']
            wbufs_alloc(2, [512, 320])
            norm_bufs(4)
            pn_bufs()
            cqn = alloc([128, 4, 512], BF16)
            S_cqn = P.slot()
            ckvn = alloc([128, 2, 512], BF16)
            S_ckvn = P.slot()
            cos2 = alloc([64, 512], F32)
            sin2 = alloc([64, 512], F32)
            S_cs2 = P.slot()
            x32 = nb['tmp'][0:64, 1, :]
            S_x32 = nb['S_tmp'][1]
            t1 = nb['tmp'][0:64, 2, :]
            S_t1 = nb['S_tmp'][2]
            t2 = nb['tmp'][0:64, 3, :]
            S_t2 = nb['S_tmp'][3]
            wuq = alloc([128, 4, 768], BF16)
            wukv = alloc([128, 2, 1024], BF16)
            S_wu = P.slot()
            P.dma('pool', lambda e: e.dma_start(out=wuq, in_=D['mla_w_uq'][l].rearrange("(kc p) n -> p kc n", p=128)),
                  writes=[S_wu])
            P.dma('pool', lambda e: e.dma_start(out=wukv, in_=D['mla_w_ukv'][l].rearrange("(kc p) n -> p kc n", p=128)),
                  writes=[S_wu])
            wa, S_wa = load_slab(w_in[:, OFF['mcq']:OFF['mcq'] + 512], 16, 512)
            wb, S_wb = load_slab(w_in[:, OFF['mckv']:OFF['mckv'] + 320], 16, 320)

            def rope(src32, S_src, out, S_out):
                bank, S_b = psB.next()
                P.op('pe', lambda e: e.matmul(bank[0:64, :], lhsT=rotT, rhs=src32, start=True, stop=True),
                     reads=[S_src, S_c], writes=[S_b])
                P.op('dve', lambda e: e.tensor_tensor(out=t1, in0=src32, in1=cos2, op=ALU.mult),
                     reads=[S_src, S_cs2], writes=[S_t1])
                P.op('dve', lambda e: e.tensor_tensor(out=t2, in0=bank[0:64, :], in1=sin2, op=ALU.mult),
                     reads=[S_b, S_cs2], writes=[S_t2])
                P.op('dve', lambda e: e.tensor_tensor(out=out, in0=t1, in1=t2, op=ALU.add),
                     reads=[S_t1, S_t2], writes=S_out)

            for tt in range(4):
                P.dma('sp', lambda e, tt=tt: e.dma_start(out=cos2, in_=cs_d[0:64, tt * 512:(tt + 1) * 512]), writes=[S_cs2])
                P.dma('sp', lambda e, tt=tt: e.dma_start(out=sin2, in_=cs_d[64:128, tt * 512:(tt + 1) * 512]), writes=[S_cs2])
                for c in range(4):
                    bank, S_b = psA.next()
                    gemm_fm(bank, S_b, 128, wa, S_wa, c * 128, tt)
                    evac(bank, S_b, nb['tmp'][:, c, :], [nb['S_tmp'][c]], eng='act' if c % 2 == 0 else 'dve')
                fm_norm(nb['tmp'], nb['S_tmp'], 128, 4, 2, lambda c: cqn[:, c, :], lambda c: [S_cqn])
                for c in range(2):
                    bank, S_b = psA.next()
                    gemm_fm(bank, S_b, 128, wb, S_wb, c * 128, tt)
                    evac(bank, S_b, nb['tmp'][:, c, :], [nb['S_tmp'][c]], eng='act' if c % 2 == 0 else 'dve')
                fm_norm(nb['tmp'], nb['S_tmp'], 128, 2, 6, lambda c: ckvn[:, c, :], lambda c: [S_ckvn])
                bank, S_b = psA.next()
                gemm_fm(bank, S_b, 64, wb, S_wb, 256, tt)
                evac(bank, S_b, nb['tmp'][0:64, 0, :], [nb['S_tmp'][0]], M=64)
                fm_norm(nb['tmp'], nb['S_tmp'], 64, 1, 11, lambda c: x32, lambda c: [S_x32])
                rope(x32, S_x32, kpe[:, tt * 512:(tt + 1) * 512], [S_kpe[tt]])
                for h in range(4):
                    proj_norm_fm(wuq, S_wu, h * 192, 128, tt, 8, b['qT'][:, h, tt * 512:(tt + 1) * 512],
                                 [b['S_q'][h][tt]], src=cqn, S_src=[S_cqn], nk=4, t0=0)
                    proj_norm_fm(wuq, S_wu, h * 192 + 128, 64, tt, 9, x32, [S_x32], src=cqn, S_src=[S_cqn], nk=4, t0=0,
                                 post=(lambda h=h, tt=tt: rope(x32, S_x32, qpe[:, h, tt * 512:(tt + 1) * 512],
                                                               [S_qpe[h][tt]])))
                    proj_norm_fm(wukv, S_wu, h * 256, 128, tt, 10, b['kT'][:, h, tt * 512:(tt + 1) * 512],
                                 [b['S_k'][h][tt]], src=ckvn, S_src=[S_ckvn], nk=2, t0=0)
                pn_flush()
                for j in range(4):
                    t = tt * 4 + j
                    bank, S_b = psA.next()
                    for h in range(4):
                        for kc in range(2):
                            P.op('pe', lambda e, bank=bank, h=h, kc=kc, j=j: e.matmul(
                                bank[:, h * 128:(h + 1) * 128], lhsT=ckvn[:, kc, j * 128:(j + 1) * 128],
                                rhs=wukv[:, kc, h * 256 + 128:h * 256 + 256], start=(kc == 0), stop=(kc == 1)),
                                reads=[S_ckvn, S_wu], writes=[S_b])
                    evac(bank, S_b, b['v'][:, t, :], [b['S_v'][t]], eng='dve')
            dbg_out('d_mq', b['qT'], [s for hh in b['S_q'] for s in hh])
            dbg_out('d_mqpe', qpe, [s for hh in S_qpe for s in hh])
            dbg_out('d_mkpe', kpe, S_kpe)
            P.barrier()
            st['off'] = keepB
            norm_bufs(0)
            attn_bufs()
            b['oT32'] = alloc([128, 4, 512], F32)
            b['S_o'] = P.slots(4)
            b['ostage'] = alloc([128, 4, 512], BF16)
            b['S_ost'] = P.slot()

            attention(range(4), 4,
                      lambda h, kt, qa, qb_, c0: [
                          (b['kT'][:, h, kt * 128:(kt + 1) * 128], b['qT'][:, h, qa:qb_],
                           [b['S_k'][h][kt // 4], b['S_q'][h][qa // 512]]),
                          (kpe[:, kt * 128:(kt + 1) * 128], qpe[:, h, qa:qb_], [S_kpe[kt // 4], S_qpe[h][qa // 512]])],
                      lambda h, kt: (b['v'][:, kt, h * 128:(h + 1) * 128], [b['S_v'][kt]]),
                      192 ** -0.5, True, 16, b['oT32'], b['S_o'],
                      tail=group_tail(1, b['oT32'], b['S_o'], b['ostage'], b['S_ost']))

        @run_if('C')
        def ph_C():
            phase()
            wbufs_alloc(2)
            norm_bufs(0)
            uT = alloc([128, 4, S], BF16)
            S_u = [P.slots(4) for _ in range(4)]
            vln = alloc([128, 16, 512], BF16)
            S_vln = P.slots(16)
            g1s = [alloc([128, 512], F32) for _ in range(2)]
            g2s = [alloc([128, 512], F32) for _ in range(2)]
            S_g1s = P.slots(2)
            S_g2s = P.slots(2)
            vgs = [alloc([128, 512], F32) for _ in range(2)]
            S_vgs = P.slots(2)
            grot = Rot([0, 1])
            lnbc = alloc([128, 1024], F32)
            bsbc = alloc([128, 512], F32)
            S_ln = P.slot()
            stats = [alloc([128, 8], F32) for _ in range(2)]
            S_stats = P.slots(2)
            ws32 = alloc([128, 4, 128], F32)
            S_ws = P.slot()
            wcT = alloc([128, 4, 128], BF16)
            S_wcT = P.slot()
            oT32 = alloc([128, 4, 512], F32)
            S_o = P.slots(4)
            ostage = alloc([128, 4, 512], BF16)
            S_ost = P.slot()
            otmp = alloc([128, 4, 128], F32)
            S_otmp = P.slots(4)
            junks = [alloc([128, 512], F32) for _ in range(2)]
            S_junks = P.slots(2)
            P.dma('sp', lambda e: e.dma_start(out=lnbc[:, 0:512], in_=D['gmlp_ln_g'][l].partition_broadcast(128)), writes=[S_ln])
            P.dma('sp', lambda e: e.dma_start(out=lnbc[:, 512:1024], in_=D['gmlp_ln_b'][l].partition_broadcast(128)), writes=[S_ln])
            P.dma('sp', lambda e: e.dma_start(out=bsbc, in_=D['gmlp_b_s'][l].rearrange("g t -> (g t)").partition_broadcast(128)),
                  writes=[S_ln])
            P.dma('sp', lambda e: e.dma_start(out=ws32, in_=D['gmlp_w_s'][l].rearrange("g t s -> t g s")), writes=[S_ws])
            for g in range(4):
                P.op('dve', lambda e, g=g: e.tensor_tensor(out=ws32[:, g, :], in0=ws32[:, g, :], in1=tril, op=ALU.mult),
                     reads=[S_ws, S_c], writes=[S_ws])
            bank, S_b = psA.next()
            for g in range(4):
                P.op('pe', lambda e, g=g, bank=bank: e.transpose(out=bank[:, g * 128:(g + 1) * 128], in_=ws32[:, g, :],
                                                                 identity=ident32), reads=[S_ws, S_c], writes=[S_b])
            P.op('dve', lambda e, bank=bank: e.tensor_copy(out=wcT, in_=bank.rearrange("p (g t) -> p g t", g=4)),
                 reads=[S_b], writes=[S_wcT])

            def gelu(src, S_src, out, S_out):
                gi_ = grot.next()
                g1, g2, S_g1, S_g2 = g1s[gi_], g2s[gi_], S_g1s[gi_], S_g2s[gi_]
                P.op('act', lambda e: e.activation(out=g1, in_=src, func=AF.Square), reads=[S_src], writes=[S_g1])
                P.op('dve', lambda e: e.tensor_scalar(out=g1, in0=g1, scalar1=0.044715, scalar2=1.0, op0=ALU.mult,
                                                      op1=ALU.add), reads=[S_g1], writes=[S_g1])
                P.op('dve', lambda e: e.tensor_tensor(out=g1, in0=src, in1=g1, op=ALU.mult), reads=[S_src, S_g1],
                     writes=[S_g1])
                P.op('act', lambda e: e.activation(out=g2, in_=g1, func=AF.Sigmoid, scale=1.5957691216057308),
                     reads=[S_g1], writes=[S_g2])
                P.op('dve', lambda e: e.tensor_tensor(out=out, in0=src, in1=g2, op=ALU.mult), reads=[S_src, S_g2],
                     writes=S_out)

            wu_, S_wu_ = load_slab(w_in[:, OFF['gu']:OFF['gu'] + 512], 16, 512)
            wv_, S_wv_ = load_slab(w_in[:, OFF['gv']:OFF['gv'] + 512], 16, 512)
            for tt in range(4):
                for c in range(4):
                    bank, S_b = psA.next()
                    gemm_fm(bank, S_b, 128, wu_, S_wu_, c * 128, tt)
                    gelu(bank, S_b, uT[:, c, tt * 512:(tt + 1) * 512], [S_u[c][tt]])
            def vln_stage(t, vg, S_vg, stat, S_stat, junk, S_junk):
                bank, S_b = psA.next()
                gemm_tm(bank, S_b, 512, wv_, S_wv_, 0, t)
                gelu(bank, S_b, vg, [S_vg])
                P.op('pool', lambda e: e.memset(stat, 0.0), writes=[S_stat])
                P.op('act', lambda e: e.activation(out=junk, in_=vg, func=AF.Identity, accum_out=stat[:, 0:1]),
                     reads=[S_vg], writes=[S_junk, S_stat])
                P.op('act', lambda e: e.activation(out=junk, in_=vg, func=AF.Square, accum_out=stat[:, 1:2]),
                     reads=[S_vg], writes=[S_junk, S_stat])
                P.op('dve', lambda e: e.tensor_scalar(out=stat[:, 2:3], in0=stat[:, 0:1], scalar1=1.0 / 512, scalar2=0.0,
                                                      op0=ALU.mult, op1=ALU.add), reads=[S_stat], writes=[S_stat])
                P.op('dve', lambda e: e.tensor_tensor(out=stat[:, 3:4], in0=stat[:, 2:3], in1=stat[:, 2:3], op=ALU.mult),
                     reads=[S_stat], writes=[S_stat])
                P.op('dve', lambda e: e.scalar_tensor_tensor(out=stat[:, 4:5], in0=stat[:, 1:2], scalar=1.0 / 512,
                                                             in1=stat[:, 3:4], op0=ALU.mult, op1=ALU.subtract),
                     reads=[S_stat], writes=[S_stat])
                P.op('act', lambda e: e.activation(out=stat[:, 5:6], in_=stat[:, 4:5], func=AF.Sqrt, bias=EPS, scale=1.0),
                     reads=[S_stat], writes=[S_stat])
                P.op('dve', lambda e: e.reciprocal(out=stat[:, 5:6], in_=stat[:, 5:6]), reads=[S_stat], writes=[S_stat])
                P.op('dve', lambda e: e.tensor_scalar(out=vg, in0=vg, scalar1=stat[:, 2:3], scalar2=stat[:, 5:6],
                                                      op0=ALU.subtract, op1=ALU.mult), reads=[S_vg, S_stat], writes=[S_vg])
                P.op('dve', lambda e: e.tensor_tensor(out=vg, in0=vg, in1=lnbc[:, 0:512], op=ALU.mult),
                     reads=[S_vg, S_ln], writes=[S_vg])
                P.op('dve', lambda e, t=t: e.tensor_tensor(out=vln[:, t, :], in0=vg, in1=lnbc[:, 512:1024], op=ALU.add),
                     reads=[S_vg, S_ln], writes=[S_vln[t]])
            for t in range(16):
                vg, S_vg, stat, S_stat, junk, S_junk = vgs[t % 2], S_vgs[t % 2], stats[t % 2], S_stats[t % 2], junks[t % 2], S_junks[t % 2]
                vln_stage(t, vg, S_vg, stat, S_stat, junk, S_junk)

            for qt in range(4):
                for g in range(4):
                    bank, S_b = psA.next()
                    for j in range(4):
                        t = qt * 4 + j
                        P.op('pe', lambda e, bank=bank, j=j, t=t, g=g: e.matmul(
                            bank[:, j * 128:(j + 1) * 128], lhsT=vln[:, t, g * 128:(g + 1) * 128], rhs=wcT[:, g, :],
                            start=True, stop=True), reads=[S_vln[t], S_wcT], writes=[S_b])
                    for j in range(4):
                        P.op('dve', lambda e, bank=bank, j=j, g=g: e.tensor_tensor(
                            out=otmp[:, j, :], in0=bank[:, j * 128:(j + 1) * 128], in1=bsbc[:, g * 128:(g + 1) * 128], op=ALU.add),
                            reads=[S_b, S_ln], writes=[S_otmp[j]])
                    P.op('dve', lambda e, g=g, qt=qt: e.tensor_tensor(
                        out=oT32[:, g, :], in0=otmp.rearrange("p a b -> p (a b)"),
                        in1=uT[:, g, qt * 512:(qt + 1) * 512], op=ALU.mult),
                        reads=S_otmp + [S_u[g][qt]], writes=[S_o[g]])
                group_tail(2, oT32, S_o, ostage, S_ost)(qt)

        @run_if('D')
        def ph_D():
            phase()
            wbufs_alloc(2)
            norm_bufs(0)
            pn_bufs()
            attn_bufs()
            b = qk_bufs()
            km32 = alloc([128, 8], F32)
            S_km32 = P.slot()
            kmean = alloc([128, 4, 8], BF16)
            S_km = P.slot()
            g8 = alloc([128, 128], F32)
            m8 = alloc([128, 128], F32)
            b8 = alloc([128, 128], F32)
            S_g8 = P.slot()
            S_m8 = P.slots(16)
            S_b8 = P.slots(16)
            biasT = alloc([8, 4, S], BF16)
            S_bT = [P.slots(4) for _ in range(4)]
            wq, S_wq = load_slab(w_in[:, OFF['bq']:OFF['bq'] + 512], 16, 512)
            wk, S_wk = load_slab(w_in[:, OFF['bk']:OFF['bk'] + 512], 16, 512)
            qk_proj(b, 'qT', 'S_q', wq, S_wq, 12)
            wv, S_wv = load_slab(w_in[:, OFF['bv']:OFF['bv'] + 512], 16, 512)
            qk_proj(b, 'kT', 'S_k', wk, S_wk, 13)
            v_proj(b, wv, S_wv, 0)
            for h in range(4):
                P.op('dve', lambda e, h=h: e.tensor_reduce(out=km32, in_=b['kT'][:, h, :].rearrange("p (n k) -> p n k", k=256),
                                                           axis=AX.X, op=ALU.add), reads=b['S_k'][h], writes=[S_km32])
                P.op('dve', lambda e, h=h: e.tensor_scalar(out=kmean[:, h, :], in0=km32, scalar1=1.0 / 256, scalar2=0.0,
                                                           op0=ALU.mult, op1=ALU.add), reads=[S_km32], writes=[S_km])

            for h in range(4):
                gb, S_gb = psA.next()
                for t in range(16):
                    P.op('pe', lambda e, gb=gb, t=t, h=h: e.matmul(gb[:, t * 8:(t + 1) * 8],
                                                                   lhsT=b['qT'][:, h, t * 128:(t + 1) * 128],
                                                                   rhs=kmean[:, h, :], start=True, stop=True),
                         reads=[b['S_q'][h][t // 4], S_km], writes=[S_gb])
                P.op('dve', lambda e, gb=gb: e.tensor_tensor(out=g8, in0=gb[:, 0:128], in1=mneg16, op=ALU.add),
                     reads=[S_gb, S_c], writes=[S_g8])
                for t in range(16):
                    P.op('dve', lambda e, t=t: e.max(out=m8[:, t * 8:(t + 1) * 8], in_=g8[:, t * 8:(t + 1) * 8]),
                         reads=[S_g8], writes=[S_m8[t]])
                    P.op('dve', lambda e, t=t: e.tensor_scalar(out=b8[:, t * 8:(t + 1) * 8], in0=g8[:, t * 8:(t + 1) * 8],
                                                               scalar1=m8[:, t * 8 + 3:t * 8 + 4], scalar2=1.0,
                                                               op0=ALU.is_ge, op1=ALU.subtract),
                         reads=[S_g8, S_m8[t]], writes=[S_b8[t]])
                for q4 in range(4):
                    tb, S_tb = psB.next()
                    for j in range(4):
                        t = q4 * 4 + j
                        P.op('pe', lambda e, tb=tb, j=j, t=t: e.transpose(out=tb[0:8, j * 128:(j + 1) * 128],
                                                                          in_=b8[:, t * 8:(t + 1) * 8], identity=ident32),
                             reads=[S_b8[t], S_c], writes=[S_tb])
                    P.op('act', lambda e, tb=tb, h=h, q4=q4: e.activation(out=biasT[:, h, q4 * 512:(q4 + 1) * 512],
                                                                          in_=tb[0:8, :], func=AF.Copy, scale=-NEGBIG),
                         reads=[S_tb], writes=[S_bT[h][q4]])

            attention(range(4), 4,
                      lambda h, kt, qa, qb_, c0: [
                          (b['kT'][:, h, kt * 128:(kt + 1) * 128], b['qT'][:, h, qa:qb_],
                           [b['S_k'][h][kt // 4], b['S_q'][h][qa // 512]]),
                          (onehot8[:, (kt // 2) * 128:(kt // 2 + 1) * 128], biasT[:, h, qa:qb_], [S_c, S_bT[h][qa // 512]])],
                      lambda h, kt: (b['v'][:, kt, h * 128:(h + 1) * 128], [b['S_v'][kt]]),
                      128 ** -0.5, True, 16, b['oT32'], b['S_o'],
                      tail=group_tail(3, b['oT32'], b['S_o'], b['ostage'], b['S_ost']))

        def accum_tile(t, gemm_for_fb, yst, S_yst, rot):
            i = rot.next()
            for fb in range(4):
                bank, S_b = psA.next()
                gemm_for_fb(fb, bank, S_b)
                evac(bank, S_b, yst[i][:, fb * 512:(fb + 1) * 512], [S_yst[i][fb]], eng='act' if fb % 2 == 0 else 'dve')
            P.dma('pool', lambda e: e.dma_start(out=y_d[t * 128:(t + 1) * 128, :], in_=yst[i], accum_op=ALU.add),
                  reads=S_yst[i], writes=S_y[t])

        @run_if('O')
        def ph_O():
            phase()
            wbufs_alloc(4)
            yst = [alloc([128, DM], F32) for _ in range(3)]
            S_yst = [P.slots(4) for _ in range(3)]
            yrot = Rot([0, 1, 2])
            for g in range(4):
                P.dma('sp', lambda e, g=g: e.dma_start(out=actT[:, g * 4:(g + 1) * 4, :],
                                                       in_=mixT_d[g * 512:(g + 1) * 512, :].rearrange("(c p) t -> p c t", p=128)),
                      reads=S_mix[g], writes=S_act)
            dbg_out('d_mixT', actT, S_act)
            wos = [load_slab(D['w_out'][l][:, fb * 512:(fb + 1) * 512], 16, 512) for fb in range(4)]
            for t in range(16):
                accum_tile(t, lambda fb, bank, S_b, t=t: gemm_tm(bank, S_b, 512, wos[fb][0], wos[fb][1], 0, t),
                           yst, S_yst, yrot)

        @run_if('X')
        def ph_X():
            phase()
            norm_T(lambda tt: y_d[tt * 128:(tt + 1) * 128, :], lambda tt: S_y[tt], D['xattn_norm'][l], actT, S_act, 16)
            phase()
            memT = alloc([128, 16, 256], BF16)
            S_memT = P.slots(2)
            keep = st['off']
            norm_T(lambda tt: D['mem'][tt * 128:(tt + 1) * 128, :], lambda tt: [], D['mem_norm'][l], memT, S_memT, 2)
            P.barrier()
            st['off'] = keep
            wbufs_alloc(2)
            norm_bufs(1)
            pn_bufs()
            attn_bufs()
            qT = alloc([128, 4, S], BF16)
            S_q = [P.slots(4) for _ in range(4)]
            xkT = alloc([128, 4, 256], BF16)
            S_xk = P.slots(4)
            xv = alloc([128, 2, 512], BF16)
            S_xv = P.slots(2)
            oT32 = alloc([128, 4, 512], F32)
            S_o = P.slots(4)
            aT = alloc([128, 4, S], BF16)
            S_a = P.slots(4)
            yst = [alloc([128, DM], F32) for _ in range(2)]
            S_yst = [P.slots(4) for _ in range(2)]
            yrot = Rot([0, 1])
            wk, S_wk = load_slab(D['w_xkv'][l][:, 0:512], 16, 512)
            wv, S_wv = load_slab(D['w_xkv'][l][:, 512:1024], 16, 512)
            for h in range(4):
                bank, S_b = psA.next()
                gemm_fm(bank, S_b, 128, wk, S_wk, h * 128, 0, width=256, src=memT, S_src=S_memT, t0=0)
                evac(bank, S_b, nb['tmp'][:, 0, 0:256], [nb['S_tmp'][0]], width=256)
                fm_norm(nb['tmp'], nb['S_tmp'], 128, 1, 31, lambda c: xkT[:, h, :], lambda c: [S_xk[h]], width=256)
            for mt in range(2):
                bank, S_b = psA.next()
                gemm_tm(bank, S_b, 512, wv, S_wv, 0, mt, src=memT, S_src=[S_memT[mt]])
                evac(bank, S_b, xv[:, mt, :], [S_xv[mt]], eng='dve')
            wq, S_wq = load_slab(D['w_xq'][l], 16, 512)
            for tt in range(4):
                for h in range(4):
                    proj_norm_fm(wq, S_wq, h * 128, 128, tt, 30, qT[:, h, tt * 512:(tt + 1) * 512], [S_q[h][tt]])
            pn_flush()

            def x_tail(qt):
                P.op('act', lambda e: e.activation(out=aT[:, :, qt * 512:(qt + 1) * 512], in_=oT32, func=AF.Copy),
                     reads=S_o, writes=[S_a[qt]])

            attention(range(4), 4,
                      lambda h, kt, qa, qb_, c0: [(xkT[:, h, kt * 128:(kt + 1) * 128], qT[:, h, qa:qb_],
                                                  [S_xk[h], S_q[h][qa // 512]])],
                      lambda h, kt: (xv[:, kt, h * 128:(h + 1) * 128], [S_xv[kt]]),
                      128 ** -0.5, False, 2, oT32, S_o, tail=x_tail)
            wo, S_wo = load_slab(D['w_xo'][l], 4, 2048, kind='rows')
            for t in range(16):
                accum_tile(t, lambda fb, bank, S_b, t=t: gemm_tm(bank, S_b, 512, wo, S_wo, fb * 512, t, src=aT,
                                                                 S_src=[S_a[t // 4]], nk=4), yst, S_yst, yrot)

        @run_if('F')
        def ph_F():
            phase()
            norm_T(lambda tt: y_d[tt * 128:(tt + 1) * 128, :], lambda tt: S_y[tt], D['ffn_norm'][l], actT, S_act, 16)
            phase()
            wbufs_alloc(5)
            hT = [alloc([128, 4, S], BF16) for _ in range(2)]
            S_h = [[P.slots(4) for _ in range(4)] for _ in range(2)]
            sg = [alloc([128, 512], F32) for _ in range(2)]
            S_sg = P.slots(2)
            yst = [alloc([128, DM], F32) for _ in range(2)]
            S_yst = [P.slots(4) for _ in range(2)]
            yrot = Rot([0, 1])
            srot = Rot([0, 1])
            wgu = D['w_gate_up'][l]
            NG = DFF // 512

            def ffn_loads(gi):
                a1 = load_slab(wgu[:, gi * 512:(gi + 1) * 512], 16, 512)
                a2 = load_slab(wgu[:, DFF + gi * 512:DFF + (gi + 1) * 512], 16, 512)
                a3 = load_slab(D['w_down'][l][gi * 512:(gi + 1) * 512, :], 4, 2048, kind='rows')
                return a1 + a2 + a3

            nxt = ffn_loads(0)
            for gi in range(NG):
                hb = gi % 2
                wg, S_wg, wu_, S_wu_, wd, S_wd = nxt
                for c in range(4):
                    for tt in range(4):
                        bg, S_bg = psA.next()
                        gemm_fm(bg, S_bg, 128, wg, S_wg, c * 128, tt)
                        bu, S_bu = psB.next()
                        gemm_fm(bu, S_bu, 128, wu_, S_wu_, c * 128, tt)
                        si = srot.next()
                        P.op('act', lambda e, bg=bg, si=si: e.activation(out=sg[si], in_=bg, func=AF.Silu),
                             reads=[S_bg], writes=[S_sg[si]])
                        P.op('dve', lambda e, bu=bu, si=si, c=c, tt=tt, hb=hb: e.tensor_tensor(
                            out=hT[hb][:, c, tt * 512:(tt + 1) * 512], in0=bu, in1=sg[si], op=ALU.mult),
                            reads=[S_bu, S_sg[si]], writes=[S_h[hb][c][tt]])
                if gi + 1 < NG:
                    nxt = ffn_loads(gi + 1)
                for t in range(16):
                    accum_tile(t, lambda fb, bank, S_b, t=t, wd=wd, S_wd=S_wd, hb=hb: gemm_tm(
                        bank, S_b, 512, wd, S_wd, fb * 512, t, src=hT[hb],
                        S_src=[S_h[hb][c][t // 4] for c in range(4)], nk=4), yst, S_yst, yrot)

    for l in range(L):
        layer(l)
    P.emit()
    es.close()
    return P


_CONSTS = None


def kernel(**inputs):
    global _CONSTS
    nc = bass.Bass("TRN2", target_bir_lowering=False)
    build(nc, L=2)
    if _CONSTS is None:
        _CONSTS = host_consts()
    shared = {}
    for name, shape in WSH:
        shared[name] = np.ascontiguousarray(np.asarray(inputs[name], dtype=np.float32))
    shared.update(_CONSTS)
    x = np.asarray(inputs['x'], dtype=np.float32)
    mem = np.asarray(inputs['mem'], dtype=np.float32)
    in_maps = []
    for b in range(8):
        m = dict(shared)
        m['x'] = np.ascontiguousarray(x[b])
        m['mem'] = np.ascontiguousarray(mem[b])
        in_maps.append(m)
    res = run_bass_kernel_spmd(nc, in_maps, core_ids=list(range(8)))
    return np.stack([np.asarray(r['y'], dtype=np.float32) for r in res.results], axis=0)
```

```python
import numpy as np
from contextlib import ExitStack
import concourse.bass as bass
import concourse.mybir as mybir
from concourse.bass_utils import run_bass_kernel_spmd

F32 = mybir.dt.float32
BF16 = mybir.dt.bfloat16
AF = mybir.ActivationFunctionType
ALU = mybir.AluOpType
AX = mybir.AxisListType

COMPUTE = ('pe', 'act', 'dve', 'pool')
NDMASEM = 8
SAME_ENGINE_SYNC = True


class Slot:
    __slots__ = ('w', 'r', 'name')

    def __init__(self, name=''):
        self.w = None
        self.r = {}
        self.name = name


class Op:
    __slots__ = ('eng', 'fn', 'deps', 'sig', 'val', 'sem', 'isdma', 'key')

    def __init__(self, eng, fn):
        self.eng = eng
        self.fn = fn
        self.deps = set()
        self.sig = False
        self.val = 0
        self.sem = None
        self.isdma = False
        self.key = eng


class Prog:
    def __init__(self, nc):
        self.nc = nc
        self.ops = {e: [] for e in COMPUTE + ('sp',)}
        self.dma_count = {'sp': 0, 'pool': 0, 'act': 0}
        self.dma_last = {}
        self.nops = 0
        self.pending = {}
        self.last_compute = {}

    def slot(self, name=''):
        return Slot(name)

    def slots(self, n, name=''):
        return [Slot(name + str(i)) for i in range(n)]

    def barrier(self):
        last = set(self.last_compute.values()) | set(self.dma_last.values())
        self.pending = {e: set(last) for e in self.ops}

    def _add(self, o, reads, writes):
        deps = o.deps
        pend = self.pending.pop(o.eng, None)
        if pend:
            deps |= pend
        if not o.isdma:
            self.last_compute[o.eng] = o
        for s in reads:
            if s.w is not None:
                deps.add(s.w)
        for s in writes:
            if s.w is not None:
                deps.add(s.w)
            for r in s.r.values():
                deps.add(r)
        for s in writes:
            s.w = o
            s.r = {}
        for s in reads:
            s.r[o.key] = o
        deps.discard(o)
        if o.eng == 'pe' and not o.isdma:
            o.deps = {d for d in deps if d.isdma or d.eng != 'pe'}
        elif not SAME_ENGINE_SYNC and not o.isdma:
            o.deps = {d for d in deps if d.isdma or d.eng != o.eng}
        self.ops[o.eng].append(o)
        self.nops += 1
        return o

    def op(self, eng, fn, reads=(), writes=()):
        return self._add(Op(eng, fn), reads, writes)

    def dma(self, eng, fn, reads=(), writes=()):
        o = Op(eng, fn)
        o.isdma = True
        k = self.dma_count[eng]
        self.dma_count[eng] = k + 1
        o.key = (eng, k % NDMASEM)
        o.sem = o.key
        o.val = 16 * (k // NDMASEM + 1)
        prev = self.dma_last.get(o.key)
        if prev is not None:
            o.deps.add(prev)
        self.dma_last[o.key] = o
        return self._add(o, reads, writes)

    def emit(self):
        nc = self.nc
        for e in self.ops:
            for o in self.ops[e]:
                for d in o.deps:
                    if not d.isdma:
                        d.sig = True
        with ExitStack() as es:
            sems = {}
            for e in COMPUTE:
                sems[e] = es.enter_context(nc.semaphore('s_' + e))
            for q in self.dma_count:
                for i in range(NDMASEM):
                    sems[(q, i)] = es.enter_context(nc.semaphore('d_%s%d' % (q, i)))
            for e in COMPUTE:
                c = 0
                for o in self.ops[e]:
                    if o.isdma:
                        continue
                    if o.sig:
                        c += 1
                        o.val = c
                        o.sem = e
            block = es.enter_context(nc.Block())
            ops = self.ops
            dma_last = self.dma_last

            def run(e, engine):
                seen = {}
                for o in ops[e]:
                    for d in o.deps:
                        if seen.get(d.sem, 0) < d.val:
                            engine.wait_ge(sems[d.sem], d.val)
                            seen[d.sem] = d.val
                    ins = o.fn(engine)
                    if o.isdma:
                        ins.then_inc(sems[o.sem], 16)
                    elif o.sig:
                        ins.then_inc(sems[e], 1)
                if e == 'sp':
                    for key, o in dma_last.items():
                        if seen.get(o.sem, 0) < o.val:
                            engine.wait_ge(sems[o.sem], o.val)

            @block.tensor
            def _(eng):
                run('pe', eng)

            @block.scalar
            def _(eng):
                run('act', eng)

            @block.vector
            def _(eng):
                run('dve', eng)

            @block.gpsimd
            def _(eng):
                run('pool', eng)

            @block.sync
            def _(eng):
                run('sp', eng)


S = 2048
DM = 2048
DFF = 5632
EPS = 1e-6
OFF = dict(fq=0, fk=512, fv=1024, ff=1536, mcq=1540, mckv=2052, mkr=2308, gu=2372, gv=2884,
           bq=3396, bk=3908, bv=4420)
WSH = [('mix_norm', (2, 2048)), ('w_in', (2, 2048, 4932)), ('fox_b_f', (2, 4)), ('fox_q_norm', (2, 128)),
       ('fox_k_norm', (2, 128)), ('mla_q_lora_norm', (2, 512)), ('mla_w_uq', (2, 512, 768)),
       ('mla_kv_lora_norm', (2, 256)), ('mla_w_ukv', (2, 256, 1024)), ('mla_q_norm', (2, 192)),
       ('mla_k_norm', (2, 192)), ('gmlp_ln_g', (2, 512)), ('gmlp_ln_b', (2, 512)),
       ('gmlp_w_s', (2, 4, 128, 128)), ('gmlp_b_s', (2, 4, 128)), ('moba_q_norm', (2, 128)),
       ('moba_k_norm', (2, 128)), ('group_norm', (2, 4, 512)), ('w_out', (2, 2048, 2048)),
       ('xattn_norm', (2, 2048)), ('mem_norm', (2, 2048)), ('w_xq', (2, 2048, 512)),
       ('w_xkv', (2, 2048, 1024)), ('xattn_q_norm', (2, 128)), ('xattn_k_norm', (2, 128)),
       ('w_xo', (2, 512, 2048)), ('ffn_norm', (2, 2048)), ('w_gate_up', (2, 2048, 11264)),
       ('w_down', (2, 5632, 2048))]
C32W = 128 * 4 + 64 + 512 + 128
CBW = 128 + 128 + 1024
NEGBIG = -30000.0


def host_consts():
    import ml_dtypes
    c32 = np.zeros((128, C32W), np.float32)
    c32[:, 0:128] = np.eye(128)
    k = np.arange(128)[:, None]
    q = np.arange(128)[None, :]
    c32[:, 128:256] = np.where(q >= k, 0.0, NEGBIG)
    c32[:, 256:384] = (q <= k).astype(np.float32)
    c32[:, 384:512] = 1.0
    rot = np.zeros((64, 64), np.float32)
    for i in range(32):
        rot[i + 32, i] = -1.0
        rot[i, i + 32] = 1.0
    c32[0:64, 512:576] = rot
    for h in range(4):
        c32[h, 576 + h * 128:576 + (h + 1) * 128] = -np.sqrt(128.0)
    mneg = np.zeros((8, 8), np.float32)
    for qb in range(8):
        for n in range(8):
            mneg[qb, n] = 0.0 if n < qb else (1e30 if n == qb else -1e30)
    c32[:, 1088:1216] = np.concatenate([mneg[t // 2] for t in range(16)]).reshape(1, 128)
    cb = np.zeros((128, CBW), np.float32)
    cb[:, 0:128] = np.eye(128)
    cb[:, 128:256] = 1.0
    for n in range(8):
        cb[n, 256 + n * 128:256 + (n + 1) * 128] = 1.0
    pos = np.arange(S, dtype=np.float32)
    inv = (np.float32(10000.0) ** (-np.arange(0, 64, 2, dtype=np.float32) / np.float32(64))).astype(np.float32)
    ang = (pos[:, None] * inv[None, :]).astype(np.float32)
    cos = np.cos(ang).astype(np.float32).T
    sin = np.sin(ang).astype(np.float32).T
    cs = np.concatenate([cos, cos, sin, sin], 0).astype(np.float32)
    return dict(c32=c32, cb=cb.astype(ml_dtypes.bfloat16), cossin=np.ascontiguousarray(cs))


class Rot:
    def __init__(self, items):
        self.items = items
        self.i = 0

    def next(self):
        it = self.items[self.i % len(self.items)]
        self.i += 1
        return it


def build(nc, L=2, phases=None, dbg=()):
    P = Prog(nc)
    es = ExitStack()
    D = {}
    for n, s in [('x', (S, DM)), ('mem', (256, DM))] + WSH:
        D[n] = nc.dram_tensor(n, list(s), F32, kind="ExternalInput").ap()
    c32_d = nc.dram_tensor("c32", [128, C32W], F32, kind="ExternalInput").ap()
    cb_d = nc.dram_tensor("cb", [128, CBW], BF16, kind="ExternalInput").ap()
    cs_d = nc.dram_tensor("cossin", [128, S], F32, kind="ExternalInput").ap()
    y_d = nc.dram_tensor("y", [S, DM], F32, kind="ExternalOutput").ap()
    mixT_d = nc.dram_tensor("mixT", [DM, S], BF16).ap()
    dbg_d = {}
    for name, shape, dt in dbg:
        dbg_d[name] = nc.dram_tensor(name, list(shape), dt, kind="ExternalOutput").ap()

    ARENA = 211200
    arena = es.enter_context(nc.sbuf_tensor("arena", [128, ARENA // 2], BF16))
    ps = [es.enter_context(nc.psum_tensor("ps%d" % i, [128, 512], F32)) for i in range(8)]
    S_ps = P.slots(8, 'ps')
    psA = Rot([(ps[i][:], S_ps[i]) for i in range(4)])
    psB = Rot([(ps[i][:], S_ps[i]) for i in range(4, 8)])
    psS = Rot([(ps[i][:], S_ps[i]) for i in range(0, 3)])
    psM = Rot([(ps[3][:], S_ps[3])])

    st = {'off': 0}

    def alloc(shape, dt, name=''):
        n = 1
        for d in shape[1:]:
            n *= d
        nb = n * (4 if dt == F32 else 2)
        nb = (nb + 63) // 64 * 64
        o = st['off']
        assert o + nb <= ARENA, (name, o, nb)
        st['off'] = o + nb
        a = arena[:, o // 2:(o + nb) // 2]
        if dt == F32:
            a = a.bitcast(F32)
        a = a[:, 0:n]
        if len(shape) == 3:
            a = a.rearrange("p (a b) -> p a b", a=shape[1])
        if shape[0] < 128:
            a = a[0:shape[0]]
        return a

    actT = alloc([128, 16, S], BF16, 'actT')
    S_act = P.slots(16, 'act')
    cb = alloc([128, CBW], BF16)
    c32 = alloc([128, C32W], F32)
    S_c = P.slot('consts')
    identb = cb[:, 0:128]
    onesb = cb[:, 128:256]
    onehot8 = cb[0:8, 256:1280]
    ident32 = c32[:, 0:128]
    maskT = c32[:, 128:256]
    tril = c32[:, 256:384]
    ones32 = c32[:, 384:512]
    rotT = c32[0:64, 512:576]
    selneg = c32[0:4, 576:1088]
    mneg16 = c32[:, 1088:1216]
    gcol = alloc([128, 40], F32)
    S_gcol = P.slot('gcol')
    ssq = alloc([128, 16], F32)
    rsq = alloc([128, 16], F32)
    S_ssq = P.slot()
    S_rsq = P.slot()
    PH0 = st['off']
    S_y = [P.slots(4, 'y%d_' % t) for t in range(16)]
    S_mix = [P.slots(4, 'mix%d_' % g) for g in range(4)]

    P.dma('sp', lambda e: e.dma_start(out=cb, in_=cb_d), writes=[S_c])
    P.dma('sp', lambda e: e.dma_start(out=c32, in_=c32_d), writes=[S_c])

    def phase():
        P.barrier()
        st['off'] = PH0

    def col_load(vec, c):
        n = vec.shape[0]
        P.dma('sp', lambda e: e.dma_start(out=gcol[0:n, c:c + 1], in_=vec.rearrange("(p o) -> p o", o=1)),
              writes=[S_gcol])

    def norm_T(src, S_src, gvec, dstT, S_dst, ntile, copy=None, S_copy=None):
        xs = [alloc([128, DM], F32) for _ in range(2)]
        S_xs = P.slots(2)
        xnb = [alloc([128, DM], BF16) for _ in range(2)]
        S_xnb = P.slots(2)
        junk = alloc([128, DM], BF16)
        S_junk = P.slot()
        gbc = alloc([128, DM], F32)
        S_gbc = P.slot('gbc')
        P.dma('sp', lambda e: e.dma_start(out=gbc, in_=gvec.partition_broadcast(128)), writes=[S_gbc])
        P.op('pool', lambda e: e.memset(ssq, 0.0), writes=[S_ssq])
        for tt in range(ntile):
            i = tt % 2
            P.dma('sp', lambda e, i=i, tt=tt: e.dma_start(out=xs[i], in_=src(tt)), reads=S_src(tt), writes=[S_xs[i]])
            if copy is not None:
                P.dma('sp', lambda e, i=i, tt=tt: e.dma_start(out=copy(tt), in_=xs[i]), reads=[S_xs[i]],
                      writes=S_copy(tt))
            P.op('act', lambda e, i=i, tt=tt: e.activation(out=junk, in_=xs[i], func=AF.Square,
                                                           accum_out=ssq[:, tt:tt + 1]),
                 reads=[S_xs[i]], writes=[S_junk, S_ssq])
            P.op('act', lambda e, tt=tt: e.activation(out=rsq[:, tt:tt + 1], in_=ssq[:, tt:tt + 1], func=AF.Sqrt,
                                                      bias=EPS, scale=1.0 / DM), reads=[S_ssq], writes=[S_rsq])
            P.op('dve', lambda e, tt=tt: e.reciprocal(out=rsq[:, tt:tt + 1], in_=rsq[:, tt:tt + 1]),
                 reads=[S_rsq], writes=[S_rsq])
            P.op('dve', lambda e, i=i, tt=tt: e.scalar_tensor_tensor(out=xnb[i], in0=xs[i], scalar=rsq[:, tt:tt + 1],
                                                                     in1=gbc, op0=ALU.mult, op1=ALU.mult),
                 reads=[S_xs[i], S_rsq, S_gbc], writes=[S_xnb[i]])
            for half in range(2):
                bank, S_b = psA.next()
                pT = bank.bitcast(BF16).rearrange("p (a b) -> p a b", a=8)
                for j in range(8):
                    kc = half * 8 + j
                    P.op('pe', lambda e, pT=pT, j=j, kc=kc, i=i: e.transpose(
                        out=pT[:, j, :], in_=xnb[i][:, kc * 128:(kc + 1) * 128], identity=identb),
                        reads=[S_xnb[i], S_c], writes=[S_b])
                eng = 'act' if half == 0 else 'dve'
                dst = dstT[:, half * 8:(half + 1) * 8, tt * 128:(tt + 1) * 128]
                if eng == 'act':
                    P.op('act', lambda e, dst=dst, pT=pT: e.activation(out=dst, in_=pT, func=AF.Copy),
                         reads=[S_b], writes=[S_dst[tt]])
                else:
                    P.op('dve', lambda e, dst=dst, pT=pT: e.tensor_copy(out=dst, in_=pT), reads=[S_b],
                         writes=[S_dst[tt]])

    wst = {}

    def wbufs_alloc(n, widths=None):
        widths = widths or [520] * n
        wst['flat'] = [alloc([128, 16 * wd], BF16) for wd in widths]
        wst['wd'] = widths
        wst['S'] = P.slots(n, 'w')
        wst['rot'] = Rot(list(range(n)))

    def load_slab(src, kch, ncols, kind='k16'):
        i = wst['rot'].next()
        flat = wst['flat'][i]
        if kind == 'k16':
            assert ncols <= wst['wd'][i]
            v = flat.rearrange("p (k n) -> p k n", k=16)
        else:
            v = flat[:, 0:8192].rearrange("p (k n) -> p k n", k=4)
        dst = v[:, 0:kch, 0:ncols]
        P.dma('pool', lambda e: e.dma_start(out=dst, in_=src.rearrange("(kc p) n -> p kc n", p=128)),
              writes=[wst['S'][i]])
        return v, wst['S'][i]

    def gemm_fm(bank, S_b, M, w, S_w, c0, tt, width=512, src=None, S_src=None, nk=16, t0=None):
        src = actT if src is None else src
        if t0 is None:
            t0 = tt * 512
        rd = [S_w] + (S_act[t0 // 128:(t0 + width + 127) // 128] if S_src is None else S_src)
        for kc in range(nk):
            P.op('pe', lambda e, kc=kc: e.matmul(bank[0:M, 0:width], lhsT=w[:, kc, c0:c0 + M],
                                                 rhs=src[:, kc, t0:t0 + width], start=(kc == 0), stop=(kc == nk - 1)),
                 reads=rd, writes=[S_b])

    def gemm_tm(bank, S_b, N, w, S_w, c0, t128, src=None, S_src=None, nk=16):
        src = actT if src is None else src
        rd = [S_w] + ([S_act[t128]] if S_src is None else S_src)
        for kc in range(nk):
            P.op('pe', lambda e, kc=kc: e.matmul(bank[:, 0:N], lhsT=src[:, kc, t128 * 128:(t128 + 1) * 128],
                                                 rhs=w[:, kc, c0:c0 + N], start=(kc == 0), stop=(kc == nk - 1)),
                 reads=rd, writes=[S_b])

    nb = {}

    def norm_bufs(ntmp=4):
        if ntmp:
            nb['tmp'] = alloc([128, ntmp, 512], F32)
        nb['S_tmp'] = P.slots(4)
        nb['sq'] = alloc([128, 4, 512], BF16)
        nb['S_sq'] = P.slot()
        nb['r32'] = alloc([128, 512], F32)
        nb['S_r32'] = P.slot()

    def rstd_from_bank(bank, S_b, M, width, nfeat, r32, S_r32):
        P.op('act', lambda e: e.activation(out=r32[0:M, 0:width], in_=bank[0:M, 0:width], func=AF.Ln, bias=EPS,
                                           scale=1.0 / nfeat), reads=[S_b], writes=[S_r32])
        P.op('act', lambda e: e.activation(out=r32[0:M, 0:width], in_=r32[0:M, 0:width], func=AF.Exp, scale=-0.5),
             reads=[S_r32], writes=[S_r32])

    def fm_norm(tmp, S_tmp, M, nch, gc0, outs, S_outs, width=512, banks=None, offload=False):
        sq, r32 = nb['sq'], nb['r32']
        S_sq, S_r32 = nb['S_sq'], nb['S_r32']
        for c in range(nch):
            if offload:
                P.op('pool', lambda e, c=c: e.tensor_tensor(out=sq[0:M, c, 0:width], in0=tmp[0:M, c, 0:width],
                                                            in1=tmp[0:M, c, 0:width], op=ALU.mult),
                     reads=[S_tmp[c]], writes=[S_sq])
            else:
                P.op('act', lambda e, c=c: e.activation(out=sq[0:M, c, 0:width], in_=tmp[0:M, c, 0:width], func=AF.Square),
                     reads=[S_tmp[c]], writes=[S_sq])
        bank, S_b = (banks or psB).next()
        for c in range(nch):
            P.op('pe', lambda e, c=c: e.matmul(bank[0:M, 0:width], lhsT=onesb[0:M, 0:M], rhs=sq[0:M, c, 0:width],
                                               start=(c == 0), stop=(c == nch - 1)),
                 reads=[S_sq, S_c], writes=[S_b])
        rstd_from_bank(bank, S_b, M, width, M * nch, r32, S_r32)
        for c in range(nch):
            o_ap = outs(c)
            P.op('dve', lambda e, c=c, o_ap=o_ap: e.scalar_tensor_tensor(out=o_ap, in0=tmp[0:M, c, 0:width],
                                                              scalar=gcol[0:M, gc0 + c:gc0 + c + 1],
                                                              in1=r32[0:M, 0:width], op0=ALU.mult, op1=ALU.mult),
                 reads=[S_tmp[c], S_r32, S_gcol], writes=S_outs(c))

    def evac(bank, S_b, dst, S_dst, eng='act', M=128, width=512):
        if eng == 'act':
            P.op('act', lambda e: e.activation(out=dst, in_=bank[0:M, 0:width], func=AF.Copy), reads=[S_b], writes=S_dst)
        else:
            P.op('dve', lambda e: e.tensor_copy(out=dst, in_=bank[0:M, 0:width]), reads=[S_b], writes=S_dst)

    pn = {'pending': None, 'i': 0, 'bufs': None}

    def pn_bufs():
        pn['bufs'] = [(alloc([128, 512], BF16), P.slot(), alloc([128, 512], F32), P.slot()) for _ in range(2)]
        pn['pending'] = None

    def pn_flush():
        if pn['pending'] is not None:
            f = pn['pending']
            pn['pending'] = None
            f()

    def proj_norm_fm(w, S_w, c0, M, tt, gc, out, S_out, src=None, S_src=None, nk=16, t0=None, width=512, post=None):
        sq, S_sq, r32, S_r32 = pn['bufs'][pn['i'] % 2]
        pn['i'] += 1
        bank, S_b = psA.next()
        gemm_fm(bank, S_b, M, w, S_w, c0, tt, width=width, src=src, S_src=S_src, nk=nk, t0=t0)
        P.op('act', lambda e: e.activation(out=sq[0:M, 0:width], in_=bank[0:M, 0:width], func=AF.Square),
             reads=[S_b], writes=[S_sq])

        def stage2():
            b2, S_b2 = psB.next()
            P.op('pe', lambda e: e.matmul(b2[0:M, 0:width], lhsT=onesb[0:M, 0:M], rhs=sq[0:M, 0:width], start=True,
                                          stop=True), reads=[S_sq, S_c], writes=[S_b2])
            rstd_from_bank(b2, S_b2, M, width, M, r32, S_r32)
            P.op('dve', lambda e: e.scalar_tensor_tensor(out=out, in0=bank[0:M, 0:width], scalar=gcol[0:M, gc:gc + 1],
                                                         in1=r32[0:M, 0:width], op0=ALU.mult, op1=ALU.mult),
                 reads=[S_b, S_r32, S_gcol], writes=S_out)
            if post is not None:
                post()

        prev = pn['pending']
        pn['pending'] = stage2
        if prev is not None:
            prev()

    ab = {}

    def attn_bufs():
        ab['pt'] = [alloc([128, 512], BF16) for _ in range(4)]
        ab['S_pt'] = P.slots(4)
        ab['rot'] = Rot([0, 1, 2, 3])
        ab['rsum'] = alloc([128, 512], F32)
        ab['S_rsum'] = P.slot()

    def attention(qts, H, terms, v_of, scale, causal, nkt_all, oT32, S_o, dve_bias=None, act_bias=None,
                  setup=None, tail=None, LA=2):
        rsum, S_rsum = ab['rsum'], ab['S_rsum']
        pairs = [(qt, h) for qt in qts for h in range(H)]
        ctx = {}

        def nkt_of(qt):
            return (4 * qt + 4) if causal else nkt_all

        def start_pair(pi_):
            qt, h = pairs[pi_]
            if setup is not None and pi_ + 1 < len(pairs):
                setup(*pairs[pi_ + 1])
            po, S_po = psB.next()
            psm, S_psm = psB.next()
            ctx[pi_] = (po, S_po, psm, S_psm)

        def qk(pi_, kt):
            qt, h = pairs[pi_]
            q0 = qt * 512
            c0 = max(0, kt * 128 - q0) if causal else 0
            sb, S_sb = psS.next()
            tl = terms(h, kt, q0 + c0, q0 + 512, c0)
            for i, (l, r, rd) in enumerate(tl):
                P.op('pe', lambda e, l=l, r=r, i=i, sb=sb, c0=c0, n=len(tl): e.matmul(
                    sb[:, c0:512], lhsT=l, rhs=r, start=(i == 0), stop=(i == n - 1)), reads=rd, writes=[S_sb])
            if causal and kt * 128 >= q0:
                P.op('dve', lambda e, sb=sb, c0=c0: e.tensor_tensor(out=sb[:, c0:c0 + 128], in0=sb[:, c0:c0 + 128],
                                                                    in1=maskT, op=ALU.add),
                     reads=[S_sb, S_c], writes=[S_sb])
            if dve_bias is not None:
                dve_bias(qt, h, kt, sb, S_sb, c0)
            pi = ab['rot'].next()
            pt, S_pt = ab['pt'][pi], ab['S_pt'][pi]
            bias, brd = act_bias(h, kt) if act_bias is not None else (0.0, [])
            P.op('act', lambda e, pt=pt, sb=sb, c0=c0, bias=bias: e.activation(
                out=pt[:, c0:512], in_=sb[:, c0:512], func=AF.Exp, bias=bias, scale=scale),
                reads=[S_sb] + brd, writes=[S_pt])
            return pt, S_pt, c0

        def pv(pi_, kt, pt, S_pt, c0):
            qt, h = pairs[pi_]
            nkt = nkt_of(qt)
            po, S_po, psm, S_psm = ctx[pi_]
            vap, vrd = v_of(h, kt)
            P.op('pe', lambda e: e.matmul(po[:, c0:512], lhsT=vap, rhs=pt[:, c0:512], start=(kt == 0),
                                          stop=(kt == nkt - 1)), reads=[S_pt] + vrd, writes=[S_po])
            P.op('pe', lambda e: e.matmul(psm[:, c0:512], lhsT=onesb, rhs=pt[:, c0:512], start=(kt == 0),
                                          stop=(kt == nkt - 1)), reads=[S_pt, S_c], writes=[S_psm])
            if kt == nkt - 1:
                P.op('act', lambda e: e.activation(out=rsum, in_=psm, func=AF.Ln), reads=[S_psm], writes=[S_rsum])
                P.op('act', lambda e: e.activation(out=rsum, in_=rsum, func=AF.Exp, scale=-1.0), reads=[S_rsum],
                     writes=[S_rsum])
                P.op('dve', lambda e: e.tensor_tensor(out=oT32[:, h, :], in0=po, in1=rsum, op=ALU.mult),
                     reads=[S_po, S_rsum], writes=[S_o[h]])
                if tail is not None and h == H - 1:
                    tail(qt)

        if setup is not None:
            setup(*pairs[0])
        pend = []
        for pi_, (qt, h) in enumerate(pairs):
            for kt in range(nkt_of(qt)):
                if kt == 0:
                    start_pair(pi_)
                pend.append((pi_, kt) + qk(pi_, kt))
                if len(pend) > LA:
                    pv(*pend.pop(0))
        while pend:
            pv(*pend.pop(0))

    tail_banks = [psM]

    def group_tail(g, oT32, S_o, ostage, S_ost):
        def tail(qt):
            fm_norm(oT32, S_o, 128, 4, 14 + g * 4, lambda c: ostage[:, c, :], lambda c: [S_ost], banks=tail_banks[0],
                    offload=True)
            dst = mixT_d[g * 512:(g + 1) * 512, qt * 512:(qt + 1) * 512].rearrange("(c p) t -> p c t", p=128)
            P.dma('sp', lambda e: e.dma_start(out=dst, in_=ostage), reads=[S_ost], writes=[S_mix[g][qt]])
        return tail

    def dbg_out(name, ap, S_rd, view=None):
        if name in dbg_d:
            d = dbg_d[name] if view is None else view(dbg_d[name])
            P.dma('sp', lambda e: e.dma_start(out=d, in_=ap), reads=S_rd)

    def run_if(name):
        def deco(f):
            if phases is None or name in phases:
                f()
            return f
        return deco

    def layer(l):
        w_in = D['w_in'][l]
        _layer_body(l, w_in)

    def _layer_body(l, w_in):
        phase()
        col_load(D['fox_q_norm'][l], 0)
        col_load(D['fox_k_norm'][l], 1)
        for c in range(4):
            col_load(D['mla_q_lora_norm'][l][c * 128:(c + 1) * 128], 2 + c)
        for c in range(2):
            col_load(D['mla_kv_lora_norm'][l][c * 128:(c + 1) * 128], 6 + c)
        col_load(D['mla_q_norm'][l][0:128], 8)
        col_load(D['mla_q_norm'][l][128:192], 9)
        col_load(D['mla_k_norm'][l][0:128], 10)
        col_load(D['mla_k_norm'][l][128:192], 11)
        col_load(D['moba_q_norm'][l], 12)
        col_load(D['moba_k_norm'][l], 13)
        for g in range(4):
            for c in range(4):
                col_load(D['group_norm'][l][g][c * 128:(c + 1) * 128], 14 + g * 4 + c)
        col_load(D['xattn_q_norm'][l], 30)
        col_load(D['xattn_k_norm'][l], 31)
        col_load(D['fox_b_f'][l], 32)
        P.op('dve', lambda e: e.tensor_scalar(out=gcol[0:4, 32:33], in0=gcol[0:4, 32:33], scalar1=-1.0, scalar2=0.0,
                                              op0=ALU.mult, op1=ALU.add), reads=[S_gcol], writes=[S_gcol])

        if l == 0:
            norm_T(lambda tt: D['x'][tt * 128:(tt + 1) * 128, :], lambda tt: [], D['mix_norm'][l], actT, S_act, 16,
                   copy=lambda tt: y_d[tt * 128:(tt + 1) * 128, :], S_copy=lambda tt: S_y[tt])
        else:
            norm_T(lambda tt: y_d[tt * 128:(tt + 1) * 128, :], lambda tt: S_y[tt], D['mix_norm'][l], actT, S_act, 16)
        if l == 0:
            dbg_out('d_xnT', actT, S_act)

        def qk_bufs():
            b = {}
            b['qT'] = alloc([128, 4, S], BF16)
            b['S_q'] = [P.slots(4) for _ in range(4)]
            b['kT'] = alloc([128, 4, S], BF16)
            b['S_k'] = [P.slots(4) for _ in range(4)]
            b['v'] = alloc([128, 16, 512], BF16)
            b['S_v'] = P.slots(16)
            b['oT32'] = alloc([128, 4, 512], F32)
            b['S_o'] = P.slots(4)
            b['ostage'] = alloc([128, 4, 512], BF16)
            b['S_ost'] = P.slot()
            return b

        def v_proj(b, wv, S_wv, c0):
            for t in range(16):
                bank, S_b = psA.next()
                gemm_tm(bank, S_b, 512, wv, S_wv, c0, t)
                evac(bank, S_b, b['v'][:, t, :], [b['S_v'][t]], eng='act' if t % 2 == 0 else 'dve')

        def qk_proj(b, key, Sk, w, S_w, gc):
            for tt in range(4):
                for h in range(4):
                    proj_norm_fm(w, S_w, h * 128, 128, tt, gc, b[key][:, h, tt * 512:(tt + 1) * 512], [b[Sk][h][tt]])
            pn_flush()

        @run_if('A')
        def ph_A():
            phase()
            wbufs_alloc(2)
            norm_bufs(0)
            pn_bufs()
            attn_bufs()
            b = qk_bufs()
            sp32 = alloc([4, 512], F32)
            cs = alloc([4, S], F32)
            S_sp = P.slot()
            S_cs = P.slot()
            ones4 = alloc([4, 512], F32)
            S_ones4 = P.slot()
            ckey = alloc([128, 64], F32)
            S_ckey = P.slot()
            dqbcs = [alloc([128, 512], F32) for _ in range(2)]
            S_dqs = P.slots(2)
            dqmap = {}
            etmp = alloc([4, 512], F32)
            S_et = P.slot()
            P.op('pool', lambda e: e.memset(ones4, 1.0), writes=[S_ones4])
            wq, S_wq = load_slab(w_in[:, OFF['fq']:OFF['fq'] + 512], 16, 512)
            wk, S_wk = load_slab(w_in[:, OFF['fk']:OFF['fk'] + 512], 16, 512)
            qk_proj(b, 'qT', 'S_q', wq, S_wq, 0)
            wv, S_wv = load_slab(w_in[:, OFF['fv']:OFF['fv'] + 516], 16, 516)
            qk_proj(b, 'kT', 'S_k', wk, S_wk, 1)
            v_proj(b, wv, S_wv, 0)
            for tt in range(4):
                bank, S_b = psA.next()
                gemm_fm(bank, S_b, 4, wv, S_wv, 512, tt)
                P.op('act', lambda e, bank=bank: e.activation(out=etmp, in_=bank[0:4, :], func=AF.Exp,
                                                              bias=gcol[0:4, 32:33], scale=-1.0),
                     reads=[S_b, S_gcol], writes=[S_et])
                P.op('act', lambda e, tt=tt: e.activation(out=sp32, in_=etmp, func=AF.Ln,
                                                          bias=1.0, scale=1.0), reads=[S_et], writes=[S_sp])
                init = 0.0 if tt == 0 else cs[:, tt * 512 - 1:tt * 512]
                P.op('dve', lambda e, tt=tt, init=init: e.tensor_tensor_scan(
                    out=cs[:, tt * 512:(tt + 1) * 512], data0=ones4, data1=sp32,
                    initial=init, op0=ALU.mult, op1=ALU.add), reads=[S_sp, S_ones4, S_cs], writes=[S_cs])
            bank, S_b = psA.next()
            for t in range(16):
                P.op('pe', lambda e, t=t, bank=bank: e.transpose(out=bank[:, t * 4:(t + 1) * 4],
                                                                 in_=cs[:, t * 128:(t + 1) * 128],
                                                                 identity=ident32[0:4, 0:4]),
                     reads=[S_cs, S_c], writes=[S_b])
            P.op('dve', lambda e, bank=bank: e.tensor_copy(out=ckey, in_=bank[:, 0:64]), reads=[S_b], writes=[S_ckey])
            dbg_out('d_cs', cs, [S_cs])
            dbg_out('d_fq', b['qT'], [s for hh in b['S_q'] for s in hh])

            def fox_setup(qt, h):
                di = len(dqmap) % 2
                dqmap[(qt, h)] = di
                dqbc, S_dq = dqbcs[di], S_dqs[di]
                bank, S_b = psM.next()
                P.op('pe', lambda e: e.matmul(bank, lhsT=selneg[:, h * 128:(h + 1) * 128],
                                              rhs=cs[:, qt * 512:(qt + 1) * 512], start=True, stop=True),
                     reads=[S_cs, S_c], writes=[S_b])
                P.op('act', lambda e: e.activation(out=dqbc, in_=bank, func=AF.Copy), reads=[S_b], writes=[S_dq])

            def fox_dve_bias(qt, h, kt, sb, S_sb, c0):
                di = dqmap[(qt, h)]
                dqbc, S_dq = dqbcs[di], S_dqs[di]
                P.op('dve', lambda e: e.tensor_tensor(out=sb[:, c0:512], in0=sb[:, c0:512], in1=dqbc[:, c0:512],
                                                      op=ALU.add), reads=[S_sb, S_dq], writes=[S_sb])

            attention(range(4), 4,
                      lambda h, kt, qa, qb_, c0: [(b['kT'][:, h, kt * 128:(kt + 1) * 128], b['qT'][:, h, qa:qb_],
                                                  [b['S_k'][h][kt // 4], b['S_q'][h][qa // 512]])],
                      lambda h, kt: (b['v'][:, kt, h * 128:(h + 1) * 128], [b['S_v'][kt]]),
                      128 ** -0.5, True, 16, b['oT32'], b['S_o'],
                      dve_bias=fox_dve_bias,
                      act_bias=lambda h, kt: (ckey[:, kt * 4 + h:kt * 4 + h + 1], [S_ckey]),
                      setup=fox_setup, tail=group_tail(0, b['oT32'], b['S_o'], b['ostage'], b['S_ost']))

        @run_if('B')
        def ph_B():
            phase()
            b = {}
            b['qT'] = alloc([128, 4, S], BF16)
            b['S_q'] = [P.slots(4) for _ in range(4)]
            b['kT'] = alloc([128, 4, S], BF16)
            b['S_k'] = [P.slots(4) for _ in range(4)]
            b['v'] = alloc([128, 16, 512], BF16)
            b['S_v'] = P.slots(16)
            qpe = alloc([64, 4, S], BF16)
            S_qpe = [P.slots(4) for _ in range(4)]
            kpe = alloc([64, S], BF16)
            S_kpe = P.slots(4)
            keepB = st['off']
            wbufs_alloc(2, [512, 320])
            norm_bufs(4)
            pn_bufs()
            cqn = alloc([128, 4, 512], BF16)
            S_cqn = P.slot()
            ckvn = alloc([128, 2, 512], BF16)
            S_ckvn = P.slot()
            cos2 = alloc([64, 512], F32)
            sin2 = alloc([64, 512], F32)
            S_cs2 = P.slot()
            x32 = nb['tmp'][0:64, 1, :]
            S_x32 = nb['S_tmp'][1]
            t1 = nb['tmp'][0:64, 2, :]
            S_t1 = nb['S_tmp'][2]
            t2 = nb['tmp'][0:64, 3, :]
            S_t2 = nb['S_tmp'][3]
            wuq = alloc([128, 4, 768], BF16)
            wukv = alloc([128, 2, 1024], BF16)
            S_wu = P.slot()
            P.dma('pool', lambda e: e.dma_start(out=wuq, in_=D['mla_w_uq'][l].rearrange("(kc p) n -> p kc n", p=128)),
                  writes=[S_wu])
            P.dma('pool', lambda e: e.dma_start(out=wukv, in_=D['mla_w_ukv'][l].rearrange("(kc p) n -> p kc n", p=128)),
                  writes=[S_wu])
            wa, S_wa = load_slab(w_in[:, OFF['mcq']:OFF['mcq'] + 512], 16, 512)
            wb, S_wb = load_slab(w_in[:, OFF['mckv']:OFF['mckv'] + 320], 16, 320)

            def rope(src32, S_src, out, S_out):
                bank, S_b = psB.next()
                P.op('pe', lambda e: e.matmul(bank[0:64, :], lhsT=rotT, rhs=src32, start=True, stop=True),
                     reads=[S_src, S_c], writes=[S_b])
                P.op('dve', lambda e: e.tensor_tensor(out=t1, in0=src32, in1=cos2, op=ALU.mult),
                     reads=[S_src, S_cs2], writes=[S_t1])
                P.op('dve', lambda e: e.tensor_tensor(out=t2, in0=bank[0:64, :], in1=sin2, op=ALU.mult),
                     reads=[S_b, S_cs2], writes=[S_t2])
                P.op('dve', lambda e: e.tensor_tensor(out=out, in0=t1, in1=t2, op=ALU.add),
                     reads=[S_t1, S_t2], writes=S_out)

            for tt in range(4):
                P.dma('sp', lambda e, tt=tt: e.dma_start(out=cos2, in_=cs_d[0:64, tt * 512:(tt + 1) * 512]), writes=[S_cs2])
                P.dma('sp', lambda e, tt=tt: e.dma_start(out=sin2, in_=cs_d[64:128, tt * 512:(tt + 1) * 512]), writes=[S_cs2])
                for c in range(4):
                    bank, S_b = psA.next()
                    gemm_fm(bank, S_b, 128, wa, S_wa, c * 128, tt)
                    evac(bank, S_b, nb['tmp'][:, c, :], [nb['S_tmp'][c]], eng='act' if c % 2 == 0 else 'dve')
                fm_norm(nb['tmp'], nb['S_tmp'], 128, 4, 2, lambda c: cqn[:, c, :], lambda c: [S_cqn])
                for c in range(2):
                    bank, S_b = psA.next()
                    gemm_fm(bank, S_b, 128, wb, S_wb, c * 128, tt)
                    evac(bank, S_b, nb['tmp'][:, c, :], [nb['S_tmp'][c]], eng='act' if c % 2 == 0 else 'dve')
                fm_norm(nb['tmp'], nb['S_tmp'], 128, 2, 6, lambda c: ckvn[:, c, :], lambda c: [S_ckvn])
                bank, S_b = psA.next()
                gemm_fm(bank, S_b, 64, wb, S_wb, 256, tt)
                evac(bank, S_b, nb['tmp'][0:64, 0, :], [nb['S_tmp'][0]], M=64)
                fm_norm(nb['tmp'], nb['S_tmp'], 64, 1, 11, lambda c: x32, lambda c: [S_x32])
                rope(x32, S_x32, kpe[:, tt * 512:(tt + 1) * 512], [S_kpe[tt]])
                for h in range(4):
                    proj_norm_fm(wuq, S_wu, h * 192, 128, tt, 8, b['qT'][:, h, tt * 512:(tt + 1) * 512],
                                 [b['S_q'][h][tt]], src=cqn, S_src=[S_cqn], nk=4, t0=0)
                    proj_norm_fm(wuq, S_wu, h * 192 + 128, 64, tt, 9, x32, [S_x32], src=cqn, S_src=[S_cqn], nk=4, t0=0,
                                 post=(lambda h=h, tt=tt: rope(x32, S_x32, qpe[:, h, tt * 512:(tt + 1) * 512],
                                                               [S_qpe[h][tt]])))
                    proj_norm_fm(wukv, S_wu, h * 256, 128, tt, 10, b['kT'][:, h, tt * 512:(tt + 1) * 512],
                                 [b['S_k'][h][tt]], src=ckvn, S_src=[S_ckvn], nk=2, t0=0)
                pn_flush()
                for j in range(4):
                    t = tt * 4 + j
                    bank, S_b = psA.next()
                    for h in range(4):
                        for kc in range(2):
                            P.op('pe', lambda e, bank=bank, h=h, kc=kc, j=j: e.matmul(
                                bank[:, h * 128:(h + 1) * 128], lhsT=ckvn[:, kc, j * 128:(j + 1) * 128],
                                rhs=wukv[:, kc, h * 256 + 128:h * 256 + 256], start=(kc == 0), stop=(kc == 1)),
                                reads=[S_ckvn, S_wu], writes=[S_b])
                    evac(bank, S_b, b['v'][:, t, :], [b['S_v'][t]], eng='dve')
            dbg_out('d_mq', b['qT'], [s for hh in b['S_q'] for s in hh])
            dbg_out('d_mqpe', qpe, [s for hh in S_qpe for s in hh])
            dbg_out('d_mkpe', kpe, S_kpe)
            P.barrier()
            st['off'] = keepB
            norm_bufs(0)
            attn_bufs()
            b['oT32'] = alloc([128, 4, 512], F32)
            b['S_o'] = P.slots(4)
            b['ostage'] = alloc([128, 4, 512], BF16)
            b['S_ost'] = P.slot()

            attention(range(4), 4,
                      lambda h, kt, qa, qb_, c0: [
                          (b['kT'][:, h, kt * 128:(kt + 1) * 128], b['qT'][:, h, qa:qb_],
                           [b['S_k'][h][kt // 4], b['S_q'][h][qa // 512]]),
                          (kpe[:, kt * 128:(kt + 1) * 128], qpe[:, h, qa:qb_], [S_kpe[kt // 4], S_qpe[h][qa // 512]])],
                      lambda h, kt: (b['v'][:, kt, h * 128:(h + 1) * 128], [b['S_v'][kt]]),
                      192 ** -0.5, True, 16, b['oT32'], b['S_o'],
                      tail=group_tail(1, b['oT32'], b['S_o'], b['ostage'], b['S_ost']))

        @run_if('C')
        def ph_C():
            phase()
            wbufs_alloc(2)
            norm_bufs(0)
            uT = alloc([128, 4, S], BF16)
            S_u = [P.slots(4) for _ in range(4)]
            vln = alloc([128, 16, 512], BF16)
            S_vln = P.slots(16)
            g1s = [alloc([128, 512], F32) for _ in range(2)]
            g2s = [alloc([128, 512], F32) for _ in range(2)]
            S_g1s = P.slots(2)
            S_g2s = P.slots(2)
            vgs = [alloc([128, 512], F32) for _ in range(2)]
            S_vgs = P.slots(2)
            grot = Rot([0, 1])
            lnbc = alloc([128, 1024], F32)
            bsbc = alloc([128, 512], F32)
            S_ln = P.slot()
            stats = [alloc([128, 8], F32) for _ in range(2)]
            S_stats = P.slots(2)
            ws32 = alloc([128, 4, 128], F32)
            S_ws = P.slot()
            wcT = alloc([128, 4, 128], BF16)
            S_wcT = P.slot()
            oT32 = alloc([128, 4, 512], F32)
            S_o = P.slots(4)
            ostage = alloc([128, 4, 512], BF16)
            S_ost = P.slot()
            otmp = alloc([128, 4, 128], F32)
            S_otmp = P.slots(4)
            junks = [alloc([128, 512], F32) for _ in range(2)]
            S_junks = P.slots(2)
            P.dma('sp', lambda e: e.dma_start(out=lnbc[:, 0:512], in_=D['gmlp_ln_g'][l].partition_broadcast(128)), writes=[S_ln])
            P.dma('sp', lambda e: e.dma_start(out=lnbc[:, 512:1024], in_=D['gmlp_ln_b'][l].partition_broadcast(128)), writes=[S_ln])
            P.dma('sp', lambda e: e.dma_start(out=bsbc, in_=D['gmlp_b_s'][l].rearrange("g t -> (g t)").partition_broadcast(128)),
                  writes=[S_ln])
            P.dma('sp', lambda e: e.dma_start(out=ws32, in_=D['gmlp_w_s'][l].rearrange("g t s -> t g s")), writes=[S_ws])
            for g in range(4):
                P.op('dve', lambda e, g=g: e.tensor_tensor(out=ws32[:, g, :], in0=ws32[:, g, :], in1=tril, op=ALU.mult),
                     reads=[S_ws, S_c], writes=[S_ws])
            bank, S_b = psA.next()
            for g in range(4):
                P.op('pe', lambda e, g=g, bank=bank: e.transpose(out=bank[:, g * 128:(g + 1) * 128], in_=ws32[:, g, :],
                                                                 identity=ident32), reads=[S_ws, S_c], writes=[S_b])
            P.op('dve', lambda e, bank=bank: e.tensor_copy(out=wcT, in_=bank.rearrange("p (g t) -> p g t", g=4)),
                 reads=[S_b], writes=[S_wcT])

            def gelu(src, S_src, out, S_out):
                gi_ = grot.next()
                g1, g2, S_g1, S_g2 = g1s[gi_], g2s[gi_], S_g1s[gi_], S_g2s[gi_]
                P.op('act', lambda e: e.activation(out=g1, in_=src, func=AF.Square), reads=[S_src], writes=[S_g1])
                P.op('dve', lambda e: e.tensor_scalar(out=g1, in0=g1, scalar1=0.044715, scalar2=1.0, op0=ALU.mult,
                                                      op1=ALU.add), reads=[S_g1], writes=[S_g1])
                P.op('dve', lambda e: e.tensor_tensor(out=g1, in0=src, in1=g1, op=ALU.mult), reads=[S_src, S_g1],
                     writes=[S_g1])
                P.op('act', lambda e: e.activation(out=g2, in_=g1, func=AF.Sigmoid, scale=1.5957691216057308),
                     reads=[S_g1], writes=[S_g2])
                P.op('dve', lambda e: e.tensor_tensor(out=out, in0=src, in1=g2, op=ALU.mult), reads=[S_src, S_g2],
                     writes=S_out)

            wu_, S_wu_ = load_slab(w_in[:, OFF['gu']:OFF['gu'] + 512], 16, 512)
            wv_, S_wv_ = load_slab(w_in[:, OFF['gv']:OFF['gv'] + 512], 16, 512)
            for tt in range(4):
                for c in range(4):
                    bank, S_b = psA.next()
                    gemm_fm(bank, S_b, 128, wu_, S_wu_, c * 128, tt)
                    gelu(bank, S_b, uT[:, c, tt * 512:(tt + 1) * 512], [S_u[c][tt]])
            def vln_part1(t, vg, S_vg):
                bank, S_b = psA.next()
                gemm_tm(bank, S_b, 512, wv_, S_wv_, 0, t)
                gelu(bank, S_b, vg, [S_vg])

            def vln_stage(t, vg, S_vg, stat, S_stat, junk, S_junk):
                P.op('pool', lambda e: e.memset(stat, 0.0), writes=[S_stat])
                P.op('act', lambda e: e.activation(out=junk, in_=vg, func=AF.Identity, accum_out=stat[:, 0:1]),
                     reads=[S_vg], writes=[S_junk, S_stat])
                P.op('act', lambda e: e.activation(out=junk, in_=vg, func=AF.Square, accum_out=stat[:, 1:2]),
                     reads=[S_vg], writes=[S_junk, S_stat])
                P.op('dve', lambda e: e.tensor_scalar(out=stat[:, 2:3], in0=stat[:, 0:1], scalar1=1.0 / 512, scalar2=0.0,
                                                      op0=ALU.mult, op1=ALU.add), reads=[S_stat], writes=[S_stat])
                P.op('dve', lambda e: e.tensor_tensor(out=stat[:, 3:4], in0=stat[:, 2:3], in1=stat[:, 2:3], op=ALU.mult),
                     reads=[S_stat], writes=[S_stat])
                P.op('dve', lambda e: e.scalar_tensor_tensor(out=stat[:, 4:5], in0=stat[:, 1:2], scalar=1.0 / 512,
                                                             in1=stat[:, 3:4], op0=ALU.mult, op1=ALU.subtract),
                     reads=[S_stat], writes=[S_stat])
                P.op('act', lambda e: e.activation(out=stat[:, 5:6], in_=stat[:, 4:5], func=AF.Sqrt, bias=EPS, scale=1.0),
                     reads=[S_stat], writes=[S_stat])
                P.op('dve', lambda e: e.reciprocal(out=stat[:, 5:6], in_=stat[:, 5:6]), reads=[S_stat], writes=[S_stat])
                P.op('dve', lambda e: e.tensor_scalar(out=vg, in0=vg, scalar1=stat[:, 2:3], scalar2=stat[:, 5:6],
                                                      op0=ALU.subtract, op1=ALU.mult), reads=[S_vg, S_stat], writes=[S_vg])
                P.op('dve', lambda e: e.tensor_tensor(out=vg, in0=vg, in1=lnbc[:, 0:512], op=ALU.mult),
                     reads=[S_vg, S_ln], writes=[S_vg])
                P.op('dve', lambda e, t=t: e.tensor_tensor(out=vln[:, t, :], in0=vg, in1=lnbc[:, 512:1024], op=ALU.add),
                     reads=[S_vg, S_ln], writes=[S_vln[t]])
            vln_part1(0, vgs[0], S_vgs[0])
            for t in range(16):
                if t + 1 < 16:
                    vln_part1(t + 1, vgs[(t + 1) % 2], S_vgs[(t + 1) % 2])
                vg, S_vg, stat, S_stat, junk, S_junk = vgs[t % 2], S_vgs[t % 2], stats[t % 2], S_stats[t % 2], junks[t % 2], S_junks[t % 2]
                vln_stage(t, vg, S_vg, stat, S_stat, junk, S_junk)

            for qt in range(4):
                for g in range(4):
                    bank, S_b = psA.next()
                    for j in range(4):
                        t = qt * 4 + j
                        P.op('pe', lambda e, bank=bank, j=j, t=t, g=g: e.matmul(
                            bank[:, j * 128:(j + 1) * 128], lhsT=vln[:, t, g * 128:(g + 1) * 128], rhs=wcT[:, g, :],
                            start=True, stop=True), reads=[S_vln[t], S_wcT], writes=[S_b])
                    for j in range(4):
                        P.op('dve', lambda e, bank=bank, j=j, g=g: e.tensor_tensor(
                            out=otmp[:, j, :], in0=bank[:, j * 128:(j + 1) * 128], in1=bsbc[:, g * 128:(g + 1) * 128], op=ALU.add),
                            reads=[S_b, S_ln], writes=[S_otmp[j]])
                    P.op('dve', lambda e, g=g, qt=qt: e.tensor_tensor(
                        out=oT32[:, g, :], in0=otmp.rearrange("p a b -> p (a b)"),
                        in1=uT[:, g, qt * 512:(qt + 1) * 512], op=ALU.mult),
                        reads=S_otmp + [S_u[g][qt]], writes=[S_o[g]])
                group_tail(2, oT32, S_o, ostage, S_ost)(qt)

        @run_if('D')
        def ph_D():
            phase()
            wbufs_alloc(2)
            norm_bufs(0)
            pn_bufs()
            attn_bufs()
            b = qk_bufs()
            km32 = alloc([128, 8], F32)
            S_km32 = P.slot()
            kmean = alloc([128, 4, 8], BF16)
            S_km = P.slot()
            g8 = alloc([128, 128], F32)
            m8 = alloc([128, 128], F32)
            b8 = alloc([128, 128], F32)
            S_g8 = P.slot()
            S_m8 = P.slots(16)
            S_b8 = P.slots(16)
            biasT = alloc([8, 4, S], BF16)
            S_bT = [P.slots(4) for _ in range(4)]
            wq, S_wq = load_slab(w_in[:, OFF['bq']:OFF['bq'] + 512], 16, 512)
            wk, S_wk = load_slab(w_in[:, OFF['bk']:OFF['bk'] + 512], 16, 512)
            qk_proj(b, 'qT', 'S_q', wq, S_wq, 12)
            wv, S_wv = load_slab(w_in[:, OFF['bv']:OFF['bv'] + 512], 16, 512)
            qk_proj(b, 'kT', 'S_k', wk, S_wk, 13)
            v_proj(b, wv, S_wv, 0)
            for h in range(4):
                P.op('dve', lambda e, h=h: e.tensor_reduce(out=km32, in_=b['kT'][:, h, :].rearrange("p (n k) -> p n k", k=256),
                                                           axis=AX.X, op=ALU.add), reads=b['S_k'][h], writes=[S_km32])
                P.op('dve', lambda e, h=h: e.tensor_scalar(out=kmean[:, h, :], in0=km32, scalar1=1.0 / 256, scalar2=0.0,
                                                           op0=ALU.mult, op1=ALU.add), reads=[S_km32], writes=[S_km])

            for h in range(4):
                gb, S_gb = psA.next()
                for t in range(16):
                    P.op('pe', lambda e, gb=gb, t=t, h=h: e.matmul(gb[:, t * 8:(t + 1) * 8],
                                                                   lhsT=b['qT'][:, h, t * 128:(t + 1) * 128],
                                                                   rhs=kmean[:, h, :], start=True, stop=True),
                         reads=[b['S_q'][h][t // 4], S_km], writes=[S_gb])
                P.op('dve', lambda e, gb=gb: e.tensor_tensor(out=g8, in0=gb[:, 0:128], in1=mneg16, op=ALU.add),
                     reads=[S_gb, S_c], writes=[S_g8])
                for t in range(16):
                    P.op('dve', lambda e, t=t: e.max(out=m8[:, t * 8:(t + 1) * 8], in_=g8[:, t * 8:(t + 1) * 8]),
                         reads=[S_g8], writes=[S_m8[t]])
                    P.op('dve', lambda e, t=t: e.tensor_scalar(out=b8[:, t * 8:(t + 1) * 8], in0=g8[:, t * 8:(t + 1) * 8],
                                                               scalar1=m8[:, t * 8 + 3:t * 8 + 4], scalar2=1.0,
                                                               op0=ALU.is_ge, op1=ALU.subtract),
                         reads=[S_g8, S_m8[t]], writes=[S_b8[t]])
                for q4 in range(4):
                    tb, S_tb = psB.next()
                    for j in range(4):
                        t = q4 * 4 + j
                        P.op('pe', lambda e, tb=tb, j=j, t=t: e.transpose(out=tb[0:8, j * 128:(j + 1) * 128],
                                                                          in_=b8[:, t * 8:(t + 1) * 8], identity=ident32),
                             reads=[S_b8[t], S_c], writes=[S_tb])
                    P.op('act', lambda e, tb=tb, h=h, q4=q4: e.activation(out=biasT[:, h, q4 * 512:(q4 + 1) * 512],
                                                                          in_=tb[0:8, :], func=AF.Copy, scale=-NEGBIG),
                         reads=[S_tb], writes=[S_bT[h][q4]])

            attention(range(4), 4,
                      lambda h, kt, qa, qb_, c0: [
                          (b['kT'][:, h, kt * 128:(kt + 1) * 128], b['qT'][:, h, qa:qb_],
                           [b['S_k'][h][kt // 4], b['S_q'][h][qa // 512]]),
                          (onehot8[:, (kt // 2) * 128:(kt // 2 + 1) * 128], biasT[:, h, qa:qb_], [S_c, S_bT[h][qa // 512]])],
                      lambda h, kt: (b['v'][:, kt, h * 128:(h + 1) * 128], [b['S_v'][kt]]),
                      128 ** -0.5, True, 16, b['oT32'], b['S_o'],
                      tail=group_tail(3, b['oT32'], b['S_o'], b['ostage'], b['S_ost']))

        def accum_tile(t, gemm_for_fb, yst, S_yst, rot):
            i = rot.next()
            for fb in range(4):
                bank, S_b = psA.next()
                gemm_for_fb(fb, bank, S_b)
                evac(bank, S_b, yst[i][:, fb * 512:(fb + 1) * 512], [S_yst[i][fb]], eng='act' if fb % 2 == 0 else 'dve')
            P.dma('pool', lambda e: e.dma_start(out=y_d[t * 128:(t + 1) * 128, :], in_=yst[i], accum_op=ALU.add),
                  reads=S_yst[i], writes=S_y[t])

        @run_if('O')
        def ph_O():
            phase()
            wbufs_alloc(4)
            yst = [alloc([128, DM], F32) for _ in range(3)]
            S_yst = [P.slots(4) for _ in range(3)]
            yrot = Rot([0, 1, 2])
            for g in range(4):
                P.dma('sp', lambda e, g=g: e.dma_start(out=actT[:, g * 4:(g + 1) * 4, :],
                                                       in_=mixT_d[g * 512:(g + 1) * 512, :].rearrange("(c p) t -> p c t", p=128)),
                      reads=S_mix[g], writes=S_act)
            dbg_out('d_mixT', actT, S_act)
            wos = [load_slab(D['w_out'][l][:, fb * 512:(fb + 1) * 512], 16, 512) for fb in range(4)]
            for t in range(16):
                accum_tile(t, lambda fb, bank, S_b, t=t: gemm_tm(bank, S_b, 512, wos[fb][0], wos[fb][1], 0, t),
                           yst, S_yst, yrot)

        @run_if('X')
        def ph_X():
            phase()
            norm_T(lambda tt: y_d[tt * 128:(tt + 1) * 128, :], lambda tt: S_y[tt], D['xattn_norm'][l], actT, S_act, 16)
            phase()
            memT = alloc([128, 16, 256], BF16)
            S_memT = P.slots(2)
            keep = st['off']
            norm_T(lambda tt: D['mem'][tt * 128:(tt + 1) * 128, :], lambda tt: [], D['mem_norm'][l], memT, S_memT, 2)
            P.barrier()
            st['off'] = keep
            wbufs_alloc(2)
            norm_bufs(1)
            pn_bufs()
            attn_bufs()
            qT = alloc([128, 4, S], BF16)
            S_q = [P.slots(4) for _ in range(4)]
            xkT = alloc([128, 4, 256], BF16)
            S_xk = P.slots(4)
            xv = alloc([128, 2, 512], BF16)
            S_xv = P.slots(2)
            oT32 = alloc([128, 4, 512], F32)
            S_o = P.slots(4)
            aT = alloc([128, 4, S], BF16)
            S_a = P.slots(4)
            yst = [alloc([128, DM], F32) for _ in range(2)]
            S_yst = [P.slots(4) for _ in range(2)]
            yrot = Rot([0, 1])
            wk, S_wk = load_slab(D['w_xkv'][l][:, 0:512], 16, 512)
            wv, S_wv = load_slab(D['w_xkv'][l][:, 512:1024], 16, 512)
            for h in range(4):
                bank, S_b = psA.next()
                gemm_fm(bank, S_b, 128, wk, S_wk, h * 128, 0, width=256, src=memT, S_src=S_memT, t0=0)
                evac(bank, S_b, nb['tmp'][:, 0, 0:256], [nb['S_tmp'][0]], width=256)
                fm_norm(nb['tmp'], nb['S_tmp'], 128, 1, 31, lambda c: xkT[:, h, :], lambda c: [S_xk[h]], width=256)
            for mt in range(2):
                bank, S_b = psA.next()
                gemm_tm(bank, S_b, 512, wv, S_wv, 0, mt, src=memT, S_src=[S_memT[mt]])
                evac(bank, S_b, xv[:, mt, :], [S_xv[mt]], eng='dve')
            wq, S_wq = load_slab(D['w_xq'][l], 16, 512)
            for tt in range(4):
                for h in range(4):
                    proj_norm_fm(wq, S_wq, h * 128, 128, tt, 30, qT[:, h, tt * 512:(tt + 1) * 512], [S_q[h][tt]])
            pn_flush()

            def x_tail(qt):
                P.op('act', lambda e: e.activation(out=aT[:, :, qt * 512:(qt + 1) * 512], in_=oT32, func=AF.Copy),
                     reads=S_o, writes=[S_a[qt]])

            attention(range(4), 4,
                      lambda h, kt, qa, qb_, c0: [(xkT[:, h, kt * 128:(kt + 1) * 128], qT[:, h, qa:qb_],
                                                  [S_xk[h], S_q[h][qa // 512]])],
                      lambda h, kt: (xv[:, kt, h * 128:(h + 1) * 128], [S_xv[kt]]),
                      128 ** -0.5, False, 2, oT32, S_o, tail=x_tail)
            wo, S_wo = load_slab(D['w_xo'][l], 4, 2048, kind='rows')
            for t in range(16):
                accum_tile(t, lambda fb, bank, S_b, t=t: gemm_tm(bank, S_b, 512, wo, S_wo, fb * 512, t, src=aT,
                                                                 S_src=[S_a[t // 4]], nk=4), yst, S_yst, yrot)

        @run_if('F')
        def ph_F():
            phase()
            norm_T(lambda tt: y_d[tt * 128:(tt + 1) * 128, :], lambda tt: S_y[tt], D['ffn_norm'][l], actT, S_act, 16)
            phase()
            wbufs_alloc(5)
            hT = [alloc([128, 4, S], BF16) for _ in range(2)]
            S_h = [[P.slots(4) for _ in range(4)] for _ in range(2)]
            sg = [alloc([128, 512], F32) for _ in range(2)]
            S_sg = P.slots(2)
            yst = [alloc([128, DM], F32) for _ in range(2)]
            S_yst = [P.slots(4) for _ in range(2)]
            yrot = Rot([0, 1])
            srot = Rot([0, 1])
            wgu = D['w_gate_up'][l]
            NG = DFF // 512

            def ffn_loads(gi):
                a1 = load_slab(wgu[:, gi * 512:(gi + 1) * 512], 16, 512)
                a2 = load_slab(wgu[:, DFF + gi * 512:DFF + (gi + 1) * 512], 16, 512)
                a3 = load_slab(D['w_down'][l][gi * 512:(gi + 1) * 512, :], 4, 2048, kind='rows')
                return a1 + a2 + a3

            nxt = ffn_loads(0)
            for gi in range(NG):
                hb = gi % 2
                wg, S_wg, wu_, S_wu_, wd, S_wd = nxt
                for c in range(4):
                    for tt in range(4):
                        bg, S_bg = psA.next()
                        gemm_fm(bg, S_bg, 128, wg, S_wg, c * 128, tt)
                        bu, S_bu = psB.next()
                        gemm_fm(bu, S_bu, 128, wu_, S_wu_, c * 128, tt)
                        si = srot.next()
                        P.op('act', lambda e, bg=bg, si=si: e.activation(out=sg[si], in_=bg, func=AF.Silu),
                             reads=[S_bg], writes=[S_sg[si]])
                        P.op('dve', lambda e, bu=bu, si=si, c=c, tt=tt, hb=hb: e.tensor_tensor(
                            out=hT[hb][:, c, tt * 512:(tt + 1) * 512], in0=bu, in1=sg[si], op=ALU.mult),
                            reads=[S_bu, S_sg[si]], writes=[S_h[hb][c][tt]])
                if gi + 1 < NG:
                    nxt = ffn_loads(gi + 1)
                for t in range(16):
                    accum_tile(t, lambda fb, bank, S_b, t=t, wd=wd, S_wd=S_wd, hb=hb: gemm_tm(
                        bank, S_b, 512, wd, S_wd, fb * 512, t, src=hT[hb],
                        S_src=[S_h[hb][c][t // 4] for c in range(4)], nk=4), yst, S_yst, yrot)

    for l in range(L):
        layer(l)
    P.emit()
    es.close()
    return P


_CONSTS = None


def kernel(**inputs):
    global _CONSTS
    nc = bass.Bass("TRN2", target_bir_lowering=False)
    build(nc, L=2)
    if _CONSTS is None:
        _CONSTS = host_consts()
    shared = {}
    for name, shape in WSH:
        shared[name] = np.ascontiguousarray(np.asarray(inputs[name], dtype=np.float32))
    shared.update(_CONSTS)
    x = np.asarray(inputs['x'], dtype=np.float32)
    mem = np.asarray(inputs['mem'], dtype=np.float32)
    in_maps = []
    for b in range(8):
        m = dict(shared)
        m['x'] = np.ascontiguousarray(x[b])
        m['mem'] = np.ascontiguousarray(mem[b])
        in_maps.append(m)
    res = run_bass_kernel_spmd(nc, in_maps, core_ids=list(range(8)))
    return np.stack([np.asarray(r['y'], dtype=np.float32) for r in res.results], axis=0)
```

```python
import numpy as np
from contextlib import ExitStack
import concourse.bass as bass
import concourse.mybir as mybir
from concourse.bass_utils import run_bass_kernel_spmd

F32 = mybir.dt.float32
BF16 = mybir.dt.bfloat16
AF = mybir.ActivationFunctionType
ALU = mybir.AluOpType
AX = mybir.AxisListType

COMPUTE = ('pe', 'act', 'dve', 'pool')
NDMASEM = 8
SAME_ENGINE_SYNC = True


class Slot:
    __slots__ = ('w', 'r', 'name')

    def __init__(self, name=''):
        self.w = None
        self.r = {}
        self.name = name


class Op:
    __slots__ = ('eng', 'fn', 'deps', 'sig', 'val', 'sem', 'isdma', 'key')

    def __init__(self, eng, fn):
        self.eng = eng
        self.fn = fn
        self.deps = set()
        self.sig = False
        self.val = 0
        self.sem = None
        self.isdma = False
        self.key = eng


class Prog:
    def __init__(self, nc):
        self.nc = nc
        self.ops = {e: [] for e in COMPUTE + ('sp',)}
        self.dma_count = {'sp': 0, 'pool': 0, 'act': 0}
        self.dma_last = {}
        self.nops = 0
        self.pending = {}
        self.last_compute = {}

    def slot(self, name=''):
        return Slot(name)

    def slots(self, n, name=''):
        return [Slot(name + str(i)) for i in range(n)]

    def barrier(self):
        last = set(self.last_compute.values()) | set(self.dma_last.values())
        self.pending = {e: set(last) for e in self.ops}

    def _add(self, o, reads, writes):
        deps = o.deps
        pend = self.pending.pop(o.eng, None)
        if pend:
            deps |= pend
        if not o.isdma:
            self.last_compute[o.eng] = o
        for s in reads:
            if s.w is not None:
                deps.add(s.w)
        for s in writes:
            if s.w is not None:
                deps.add(s.w)
            for r in s.r.values():
                deps.add(r)
        for s in writes:
            s.w = o
            s.r = {}
        for s in reads:
            s.r[o.key] = o
        deps.discard(o)
        if o.eng == 'pe' and not o.isdma:
            o.deps = {d for d in deps if d.isdma or d.eng != 'pe'}
        elif not SAME_ENGINE_SYNC and not o.isdma:
            o.deps = {d for d in deps if d.isdma or d.eng != o.eng}
        self.ops[o.eng].append(o)
        self.nops += 1
        return o

    def op(self, eng, fn, reads=(), writes=()):
        return self._add(Op(eng, fn), reads, writes)

    def dma(self, eng, fn, reads=(), writes=()):
        o = Op(eng, fn)
        o.isdma = True
        k = self.dma_count[eng]
        self.dma_count[eng] = k + 1
        o.key = (eng, k % NDMASEM)
        o.sem = o.key
        o.val = 16 * (k // NDMASEM + 1)
        prev = self.dma_last.get(o.key)
        if prev is not None:
            o.deps.add(prev)
        self.dma_last[o.key] = o
        return self._add(o, reads, writes)

    def emit(self):
        nc = self.nc
        for e in self.ops:
            for o in self.ops[e]:
                for d in o.deps:
                    if not d.isdma:
                        d.sig = True
        with ExitStack() as es:
            sems = {}
            for e in COMPUTE:
                sems[e] = es.enter_context(nc.semaphore('s_' + e))
            for q in self.dma_count:
                for i in range(NDMASEM):
                    sems[(q, i)] = es.enter_context(nc.semaphore('d_%s%d' % (q, i)))
            for e in COMPUTE:
                c = 0
                for o in self.ops[e]:
                    if o.isdma:
                        continue
                    if o.sig:
                        c += 1
                        o.val = c
                        o.sem = e
            block = es.enter_context(nc.Block())
            ops = self.ops
            dma_last = self.dma_last

            def run(e, engine):
                seen = {}
                for o in ops[e]:
                    for d in o.deps:
                        if seen.get(d.sem, 0) < d.val:
                            engine.wait_ge(sems[d.sem], d.val)
                            seen[d.sem] = d.val
                    ins = o.fn(engine)
                    if o.isdma:
                        ins.then_inc(sems[o.sem], 16)
                    elif o.sig:
                        ins.then_inc(sems[e], 1)
                if e == 'sp':
                    for key, o in dma_last.items():
                        if seen.get(o.sem, 0) < o.val:
                            engine.wait_ge(sems[o.sem], o.val)

            @block.tensor
            def _(eng):
                run('pe', eng)

            @block.scalar
            def _(eng):
                run('act', eng)

            @block.vector
            def _(eng):
                run('dve', eng)

            @block.gpsimd
            def _(eng):
                run('pool', eng)

            @block.sync
            def _(eng):
                run('sp', eng)


S = 2048
DM = 2048
DFF = 5632
EPS = 1e-6
OFF = dict(fq=0, fk=512, fv=1024, ff=1536, mcq=1540, mckv=2052, mkr=2308, gu=2372, gv=2884,
           bq=3396, bk=3908, bv=4420)
WSH = [('mix_norm', (2, 2048)), ('w_in', (2, 2048, 4932)), ('fox_b_f', (2, 4)), ('fox_q_norm', (2, 128)),
       ('fox_k_norm', (2, 128)), ('mla_q_lora_norm', (2, 512)), ('mla_w_uq', (2, 512, 768)),
       ('mla_kv_lora_norm', (2, 256)), ('mla_w_ukv', (2, 256, 1024)), ('mla_q_norm', (2, 192)),
       ('mla_k_norm', (2, 192)), ('gmlp_ln_g', (2, 512)), ('gmlp_ln_b', (2, 512)),
       ('gmlp_w_s', (2, 4, 128, 128)), ('gmlp_b_s', (2, 4, 128)), ('moba_q_norm', (2, 128)),
       ('moba_k_norm', (2, 128)), ('group_norm', (2, 4, 512)), ('w_out', (2, 2048, 2048)),
       ('xattn_norm', (2, 2048)), ('mem_norm', (2, 2048)), ('w_xq', (2, 2048, 512)),
       ('w_xkv', (2, 2048, 1024)), ('xattn_q_norm', (2, 128)), ('xattn_k_norm', (2, 128)),
       ('w_xo', (2, 512, 2048)), ('ffn_norm', (2, 2048)), ('w_gate_up', (2, 2048, 11264)),
       ('w_down', (2, 5632, 2048))]
C32W = 128 * 4 + 64 + 512 + 128
CBW = 128 + 128 + 1024
NEGBIG = -30000.0


def host_consts():
    import ml_dtypes
    c32 = np.zeros((128, C32W), np.float32)
    c32[:, 0:128] = np.eye(128)
    k = np.arange(128)[:, None]
    q = np.arange(128)[None, :]
    c32[:, 128:256] = np.where(q >= k, 0.0, NEGBIG)
    c32[:, 256:384] = (q <= k).astype(np.float32)
    c32[:, 384:512] = 1.0
    rot = np.zeros((64, 64), np.float32)
    for i in range(32):
        rot[i + 32, i] = -1.0
        rot[i, i + 32] = 1.0
    c32[0:64, 512:576] = rot
    for h in range(4):
        c32[h, 576 + h * 128:576 + (h + 1) * 128] = -np.sqrt(128.0)
    mneg = np.zeros((8, 8), np.float32)
    for qb in range(8):
        for n in range(8):
            mneg[qb, n] = 0.0 if n < qb else (1e30 if n == qb else -1e30)
    c32[:, 1088:1216] = np.concatenate([mneg[t // 2] for t in range(16)]).reshape(1, 128)
    cb = np.zeros((128, CBW), np.float32)
    cb[:, 0:128] = np.eye(128)
    cb[:, 128:256] = 1.0
    for n in range(8):
        cb[n, 256 + n * 128:256 + (n + 1) * 128] = 1.0
    pos = np.arange(S, dtype=np.float32)
    inv = (np.float32(10000.0) ** (-np.arange(0, 64, 2, dtype=np.float32) / np.float32(64))).astype(np.float32)
    ang = (pos[:, None] * inv[None, :]).astype(np.float32)
    cos = np.cos(ang).astype(np.float32).T
    sin = np.sin(ang).astype(np.float32).T
    cs = np.concatenate([cos, cos, sin, sin], 0).astype(np.float32)
    return dict(c32=c32, cb=cb.astype(ml_dtypes.bfloat16), cossin=np.ascontiguousarray(cs))


class Rot:
    def __init__(self, items):
        self.items = items
        self.i = 0

    def next(self):
        it = self.items[self.i % len(self.items)]
        self.i += 1
        return it


def build(nc, L=2, phases=None, dbg=()):
    P = Prog(nc)
    es = ExitStack()
    D = {}
    for n, s in [('x', (S, DM)), ('mem', (256, DM))] + WSH:
        D[n] = nc.dram_tensor(n, list(s), F32, kind="ExternalInput").ap()
    c32_d = nc.dram_tensor("c32", [128, C32W], F32, kind="ExternalInput").ap()
    cb_d = nc.dram_tensor("cb", [128, CBW], BF16, kind="ExternalInput").ap()
    cs_d = nc.dram_tensor("cossin", [128, S], F32, kind="ExternalInput").ap()
    y_d = nc.dram_tensor("y", [S, DM], F32, kind="ExternalOutput").ap()
    mixT_d = nc.dram_tensor("mixT", [DM, S], BF16).ap()
    dbg_d = {}
    for name, shape, dt in dbg:
        dbg_d[name] = nc.dram_tensor(name, list(shape), dt, kind="ExternalOutput").ap()

    ARENA = 211200
    arena = es.enter_context(nc.sbuf_tensor("arena", [128, ARENA // 2], BF16))
    ps = [es.enter_context(nc.psum_tensor("ps%d" % i, [128, 512], F32)) for i in range(8)]
    S_ps = P.slots(8, 'ps')
    psA = Rot([(ps[i][:], S_ps[i]) for i in range(4)])
    psB = Rot([(ps[i][:], S_ps[i]) for i in range(4, 8)])
    psS = Rot([(ps[i][:], S_ps[i]) for i in range(0, 4)])
    psM = Rot([(ps[4][:], S_ps[4])])
    psO = Rot([(ps[i][:], S_ps[i]) for i in (5, 6)])
    psR = Rot([(ps[7][:], S_ps[7])])

    st = {'off': 0}

    def alloc(shape, dt, name=''):
        n = 1
        for d in shape[1:]:
            n *= d
        nb = n * (4 if dt == F32 else 2)
        nb = (nb + 63) // 64 * 64
        o = st['off']
        assert o + nb <= ARENA, (name, o, nb)
        st['off'] = o + nb
        a = arena[:, o // 2:(o + nb) // 2]
        if dt == F32:
            a = a.bitcast(F32)
        a = a[:, 0:n]
        if len(shape) == 3:
            a = a.rearrange("p (a b) -> p a b", a=shape[1])
        if shape[0] < 128:
            a = a[0:shape[0]]
        return a

    actT = alloc([128, 16, S], BF16, 'actT')
    S_act = P.slots(16, 'act')
    cb = alloc([128, CBW], BF16)
    c32 = alloc([128, C32W], F32)
    S_c = P.slot('consts')
    identb = cb[:, 0:128]
    onesb = cb[:, 128:256]
    onehot8 = cb[0:8, 256:1280]
    ident32 = c32[:, 0:128]
    maskT = c32[:, 128:256]
    tril = c32[:, 256:384]
    ones32 = c32[:, 384:512]
    rotT = c32[0:64, 512:576]
    selneg = c32[0:4, 576:1088]
    mneg16 = c32[:, 1088:1216]
    gcol = alloc([128, 40], F32)
    S_gcol = P.slot('gcol')
    ssq = alloc([128, 16], F32)
    rsq = alloc([128, 16], F32)
    S_ssq = P.slot()
    S_rsq = P.slot()
    PH0 = st['off']
    S_y = [P.slots(4, 'y%d_' % t) for t in range(16)]
    S_mix = [P.slots(4, 'mix%d_' % g) for g in range(4)]

    P.dma('sp', lambda e: e.dma_start(out=cb, in_=cb_d), writes=[S_c])
    P.dma('sp', lambda e: e.dma_start(out=c32, in_=c32_d), writes=[S_c])

    def phase():
        P.barrier()
        st['off'] = PH0

    def col_load(vec, c):
        n = vec.shape[0]
        P.dma('sp', lambda e: e.dma_start(out=gcol[0:n, c:c + 1], in_=vec.rearrange("(p o) -> p o", o=1)),
              writes=[S_gcol])

    def norm_prep(gvec):
        c = {}
        c['xs'] = [alloc([128, DM], F32) for _ in range(2)]
        c['S_xs'] = P.slots(2)
        c['xnb'] = [alloc([128, DM], BF16) for _ in range(2)]
        c['S_xnb'] = P.slots(2)
        c['junk'] = alloc([128, DM], BF16)
        c['S_junk'] = P.slot()
        c['gbc'] = alloc([128, DM], F32)
        c['S_gbc'] = P.slot('gbc')
        gbc = c['gbc']
        P.dma('sp', lambda e: e.dma_start(out=gbc, in_=gvec.partition_broadcast(128)), writes=[c['S_gbc']])
        P.op('pool', lambda e: e.memset(ssq, 0.0), writes=[S_ssq])
        return c

    def norm_tile(c, tt, src, S_src, dstT, S_dst, copy=None, S_copy=None, banks=None):
        xs, S_xs, xnb, S_xnb, junk, S_junk, gbc, S_gbc = (c['xs'], c['S_xs'], c['xnb'], c['S_xnb'], c['junk'],
                                                          c['S_junk'], c['gbc'], c['S_gbc'])
        i = tt % 2
        P.dma('sp', lambda e: e.dma_start(out=xs[i], in_=src(tt)), reads=S_src(tt), writes=[S_xs[i]])
        if copy is not None:
            P.dma('sp', lambda e: e.dma_start(out=copy(tt), in_=xs[i]), reads=[S_xs[i]], writes=S_copy(tt))
        P.op('act', lambda e: e.activation(out=junk, in_=xs[i], func=AF.Square, accum_out=ssq[:, tt:tt + 1]),
             reads=[S_xs[i]], writes=[S_junk, S_ssq])
        P.op('act', lambda e: e.activation(out=rsq[:, tt:tt + 1], in_=ssq[:, tt:tt + 1], func=AF.Sqrt,
                                           bias=EPS, scale=1.0 / DM), reads=[S_ssq], writes=[S_rsq])
        P.op('dve', lambda e: e.reciprocal(out=rsq[:, tt:tt + 1], in_=rsq[:, tt:tt + 1]), reads=[S_rsq], writes=[S_rsq])
        P.op('dve', lambda e: e.scalar_tensor_tensor(out=xnb[i], in0=xs[i], scalar=rsq[:, tt:tt + 1], in1=gbc,
                                                     op0=ALU.mult, op1=ALU.mult),
             reads=[S_xs[i], S_rsq, S_gbc], writes=[S_xnb[i]])
        for half in range(2):
            bank, S_b = (banks or psA).next()
            pT = bank.bitcast(BF16).rearrange("p (a b) -> p a b", a=8)
            for j in range(8):
                kc = half * 8 + j
                P.op('pe', lambda e, pT=pT, j=j, kc=kc: e.transpose(
                    out=pT[:, j, :], in_=xnb[i][:, kc * 128:(kc + 1) * 128], identity=identb),
                    reads=[S_xnb[i], S_c], writes=[S_b])
            dst = dstT[:, half * 8:(half + 1) * 8, tt * 128:(tt + 1) * 128]
            if half == 0:
                P.op('act', lambda e, dst=dst, pT=pT: e.activation(out=dst, in_=pT, func=AF.Copy),
                     reads=[S_b], writes=[S_dst[tt]])
            else:
                P.op('dve', lambda e, dst=dst, pT=pT: e.tensor_copy(out=dst, in_=pT), reads=[S_b], writes=[S_dst[tt]])

    def norm_T(src, S_src, gvec, dstT, S_dst, ntile, copy=None, S_copy=None):
        c = norm_prep(gvec)
        for tt in range(ntile):
            norm_tile(c, tt, src, S_src, dstT, S_dst, copy=copy, S_copy=S_copy)

    wst = {}

    def wbufs_alloc(n, widths=None):
        widths = widths or [520] * n
        wst['flat'] = [alloc([128, 16 * wd], BF16) for wd in widths]
        wst['wd'] = widths
        wst['S'] = P.slots(n, 'w')
        wst['rot'] = Rot(list(range(n)))

    def load_slab(src, kch, ncols, kind='k16'):
        i = wst['rot'].next()
        flat = wst['flat'][i]
        if kind == 'k16':
            assert ncols <= wst['wd'][i]
            v = flat.rearrange("p (k n) -> p k n", k=16)
        else:
            v = flat[:, 0:8192].rearrange("p (k n) -> p k n", k=4)
        dst = v[:, 0:kch, 0:ncols]
        P.dma('pool', lambda e: e.dma_start(out=dst, in_=src.rearrange("(kc p) n -> p kc n", p=128)),
              writes=[wst['S'][i]])
        return v, wst['S'][i]

    def gemm_fm(bank, S_b, M, w, S_w, c0, tt, width=512, src=None, S_src=None, nk=16, t0=None):
        src = actT if src is None else src
        if t0 is None:
            t0 = tt * 512
        rd = [S_w] + (S_act[t0 // 128:(t0 + width + 127) // 128] if S_src is None else S_src)
        for kc in range(nk):
            P.op('pe', lambda e, kc=kc: e.matmul(bank[0:M, 0:width], lhsT=w[:, kc, c0:c0 + M],
                                                 rhs=src[:, kc, t0:t0 + width], start=(kc == 0), stop=(kc == nk - 1)),
                 reads=rd, writes=[S_b])

    def gemm_tm(bank, S_b, N, w, S_w, c0, t128, src=None, S_src=None, nk=16):
        src = actT if src is None else src
        rd = [S_w] + ([S_act[t128]] if S_src is None else S_src)
        for kc in range(nk):
            P.op('pe', lambda e, kc=kc: e.matmul(bank[:, 0:N], lhsT=src[:, kc, t128 * 128:(t128 + 1) * 128],
                                                 rhs=w[:, kc, c0:c0 + N], start=(kc == 0), stop=(kc == nk - 1)),
                 reads=rd, writes=[S_b])

    nb = {}

    def norm_bufs(ntmp=4):
        if ntmp:
            nb['tmp'] = alloc([128, ntmp, 512], F32)
        nb['S_tmp'] = P.slots(4)
        nb['sq'] = alloc([128, 4, 512], BF16)
        nb['S_sq'] = P.slot()
        nb['r32'] = alloc([128, 512], F32)
        nb['S_r32'] = P.slot()

    def rstd_from_bank(bank, S_b, M, width, nfeat, r32, S_r32):
        P.op('act', lambda e: e.activation(out=r32[0:M, 0:width], in_=bank[0:M, 0:width], func=AF.Ln, bias=EPS,
                                           scale=1.0 / nfeat), reads=[S_b], writes=[S_r32])
        P.op('act', lambda e: e.activation(out=r32[0:M, 0:width], in_=r32[0:M, 0:width], func=AF.Exp, scale=-0.5),
             reads=[S_r32], writes=[S_r32])

    def fm_norm(tmp, S_tmp, M, nch, gc0, outs, S_outs, width=512, banks=None, offload=False):
        sq, r32 = nb['sq'], nb['r32']
        S_sq, S_r32 = nb['S_sq'], nb['S_r32']
        for c in range(nch):
            if offload:
                P.op('pool', lambda e, c=c: e.tensor_tensor(out=sq[0:M, c, 0:width], in0=tmp[0:M, c, 0:width],
                                                            in1=tmp[0:M, c, 0:width], op=ALU.mult),
                     reads=[S_tmp[c]], writes=[S_sq])
            else:
                P.op('act', lambda e, c=c: e.activation(out=sq[0:M, c, 0:width], in_=tmp[0:M, c, 0:width], func=AF.Square),
                     reads=[S_tmp[c]], writes=[S_sq])
        bank, S_b = (banks or psB).next()
        for c in range(nch):
            P.op('pe', lambda e, c=c: e.matmul(bank[0:M, 0:width], lhsT=onesb[0:M, 0:M], rhs=sq[0:M, c, 0:width],
                                               start=(c == 0), stop=(c == nch - 1)),
                 reads=[S_sq, S_c], writes=[S_b])
        rstd_from_bank(bank, S_b, M, width, M * nch, r32, S_r32)
        for c in range(nch):
            o_ap = outs(c)
            P.op('dve', lambda e, c=c, o_ap=o_ap: e.scalar_tensor_tensor(out=o_ap, in0=tmp[0:M, c, 0:width],
                                                              scalar=gcol[0:M, gc0 + c:gc0 + c + 1],
                                                              in1=r32[0:M, 0:width], op0=ALU.mult, op1=ALU.mult),
                 reads=[S_tmp[c], S_r32, S_gcol], writes=S_outs(c))

    def evac(bank, S_b, dst, S_dst, eng='act', M=128, width=512):
        if eng == 'act':
            P.op('act', lambda e: e.activation(out=dst, in_=bank[0:M, 0:width], func=AF.Copy), reads=[S_b], writes=S_dst)
        else:
            P.op('dve', lambda e: e.tensor_copy(out=dst, in_=bank[0:M, 0:width]), reads=[S_b], writes=S_dst)

    pn = {'pending': None, 'i': 0, 'bufs': None}

    def pn_bufs():
        pn['bufs'] = [(alloc([128, 512], BF16), P.slot(), alloc([128, 512], F32), P.slot()) for _ in range(2)]
        pn['pending'] = None

    def pn_flush():
        if pn['pending'] is not None:
            f = pn['pending']
            pn['pending'] = None
            f()

    def proj_norm_fm(w, S_w, c0, M, tt, gc, out, S_out, src=None, S_src=None, nk=16, t0=None, width=512, post=None):
        sq, S_sq, r32, S_r32 = pn['bufs'][pn['i'] % 2]
        pn['i'] += 1
        bank, S_b = psA.next()
        gemm_fm(bank, S_b, M, w, S_w, c0, tt, width=width, src=src, S_src=S_src, nk=nk, t0=t0)
        P.op('act', lambda e: e.activation(out=sq[0:M, 0:width], in_=bank[0:M, 0:width], func=AF.Square),
             reads=[S_b], writes=[S_sq])

        def stage2():
            b2, S_b2 = psB.next()
            P.op('pe', lambda e: e.matmul(b2[0:M, 0:width], lhsT=onesb[0:M, 0:M], rhs=sq[0:M, 0:width], start=True,
                                          stop=True), reads=[S_sq, S_c], writes=[S_b2])
            rstd_from_bank(b2, S_b2, M, width, M, r32, S_r32)
            P.op('dve', lambda e: e.scalar_tensor_tensor(out=out, in0=bank[0:M, 0:width], scalar=gcol[0:M, gc:gc + 1],
                                                         in1=r32[0:M, 0:width], op0=ALU.mult, op1=ALU.mult),
                 reads=[S_b, S_r32, S_gcol], writes=S_out)
            if post is not None:
                post()

        prev = pn['pending']
        pn['pending'] = stage2
        if prev is not None:
            prev()

    ab = {}

    def attn_bufs():
        ab['pt'] = [alloc([128, 512], BF16) for _ in range(5)]
        ab['S_pt'] = P.slots(5)
        ab['rot'] = Rot([0, 1, 2, 3, 4])
        ab['rsum'] = alloc([128, 512], F32)
        ab['S_rsum'] = P.slot()

    def attention(qts, H, terms, v_of, scale, causal, nkt_all, oT32, S_o, dve_bias=None, act_bias=None,
                  setup=None, tail=None, LA=3):
        rsum, S_rsum = ab['rsum'], ab['S_rsum']
        pairs = [(qt, h) for qt in qts for h in range(H)]
        ctx = {}

        def nkt_of(qt):
            return (4 * qt + 4) if causal else nkt_all

        def start_pair(pi_):
            qt, h = pairs[pi_]
            if setup is not None and pi_ + 1 < len(pairs):
                setup(*pairs[pi_ + 1])
            po, S_po = psO.next()
            psm, S_psm = psR.next()
            ctx[pi_] = (po, S_po, psm, S_psm)

        def qk(pi_, kt):
            qt, h = pairs[pi_]
            q0 = qt * 512
            c0 = max(0, kt * 128 - q0) if causal else 0
            sb, S_sb = psS.next()
            tl = terms(h, kt, q0 + c0, q0 + 512, c0)
            for i, (l, r, rd) in enumerate(tl):
                P.op('pe', lambda e, l=l, r=r, i=i, sb=sb, c0=c0, n=len(tl): e.matmul(
                    sb[:, c0:512], lhsT=l, rhs=r, start=(i == 0), stop=(i == n - 1)), reads=rd, writes=[S_sb])
            if causal and kt * 128 >= q0:
                P.op('dve', lambda e, sb=sb, c0=c0: e.tensor_tensor(out=sb[:, c0:c0 + 128], in0=sb[:, c0:c0 + 128],
                                                                    in1=maskT, op=ALU.add),
                     reads=[S_sb, S_c], writes=[S_sb])
            if dve_bias is not None:
                dve_bias(qt, h, kt, sb, S_sb, c0)
            pi = ab['rot'].next()
            pt, S_pt = ab['pt'][pi], ab['S_pt'][pi]
            bias, brd = act_bias(h, kt) if act_bias is not None else (0.0, [])
            P.op('act', lambda e, pt=pt, sb=sb, c0=c0, bias=bias: e.activation(
                out=pt[:, c0:512], in_=sb[:, c0:512], func=AF.Exp, bias=bias, scale=scale),
                reads=[S_sb] + brd, writes=[S_pt])
            return pt, S_pt, c0

        def pv(pi_, kt, pt, S_pt, c0):
            qt, h = pairs[pi_]
            nkt = nkt_of(qt)
            po, S_po, psm, S_psm = ctx[pi_]
            vap, vrd = v_of(h, kt)
            P.op('pe', lambda e: e.matmul(po[:, c0:512], lhsT=vap, rhs=pt[:, c0:512], start=(kt == 0),
                                          stop=(kt == nkt - 1)), reads=[S_pt] + vrd, writes=[S_po])
            P.op('pe', lambda e: e.matmul(psm[:, c0:512], lhsT=onesb, rhs=pt[:, c0:512], start=(kt == 0),
                                          stop=(kt == nkt - 1)), reads=[S_pt, S_c], writes=[S_psm])
            if kt == nkt - 1:
                P.op('act', lambda e: e.activation(out=rsum, in_=psm, func=AF.Ln), reads=[S_psm], writes=[S_rsum])
                P.op('act', lambda e: e.activation(out=rsum, in_=rsum, func=AF.Exp, scale=-1.0), reads=[S_rsum],
                     writes=[S_rsum])
                P.op('dve', lambda e: e.tensor_tensor(out=oT32[:, h, :], in0=po, in1=rsum, op=ALU.mult),
                     reads=[S_po, S_rsum], writes=[S_o[h]])
                if tail is not None and h == H - 1:
                    tail(qt)

        if setup is not None:
            setup(*pairs[0])
        pend = []
        for pi_, (qt, h) in enumerate(pairs):
            for kt in range(nkt_of(qt)):
                if kt == 0:
                    start_pair(pi_)
                pend.append((pi_, kt) + qk(pi_, kt))
                if len(pend) > LA:
                    pv(*pend.pop(0))
        while pend:
            pv(*pend.pop(0))

    tail_banks = [psM]

    def group_tail(g, oT32, S_o, ostage, S_ost):
        def tail(qt):
            fm_norm(oT32, S_o, 128, 4, 14 + g * 4, lambda c: ostage[:, c, :], lambda c: [S_ost], banks=tail_banks[0],
                    offload=True)
            dst = mixT_d[g * 512:(g + 1) * 512, qt * 512:(qt + 1) * 512].rearrange("(c p) t -> p c t", p=128)
            P.dma('sp', lambda e: e.dma_start(out=dst, in_=ostage), reads=[S_ost], writes=[S_mix[g][qt]])
        return tail

    def dbg_out(name, ap, S_rd, view=None):
        if name in dbg_d:
            d = dbg_d[name] if view is None else view(dbg_d[name])
            P.dma('sp', lambda e: e.dma_start(out=d, in_=ap), reads=S_rd)

    def run_if(name):
        def deco(f):
            if phases is None or name in phases:
                f()
            return f
        return deco

    def layer(l):
        w_in = D['w_in'][l]
        _layer_body(l, w_in)

    def _layer_body(l, w_in):
        phase()
        col_load(D['fox_q_norm'][l], 0)
        col_load(D['fox_k_norm'][l], 1)
        for c in range(4):
            col_load(D['mla_q_lora_norm'][l][c * 128:(c + 1) * 128], 2 + c)
        for c in range(2):
            col_load(D['mla_kv_lora_norm'][l][c * 128:(c + 1) * 128], 6 + c)
        col_load(D['mla_q_norm'][l][0:128], 8)
        col_load(D['mla_q_norm'][l][128:192], 9)
        col_load(D['mla_k_norm'][l][0:128], 10)
        col_load(D['mla_k_norm'][l][128:192], 11)
        col_load(D['moba_q_norm'][l], 12)
        col_load(D['moba_k_norm'][l], 13)
        for g in range(4):
            for c in range(4):
                col_load(D['group_norm'][l][g][c * 128:(c + 1) * 128], 14 + g * 4 + c)
        col_load(D['xattn_q_norm'][l], 30)
        col_load(D['xattn_k_norm'][l], 31)
        col_load(D['fox_b_f'][l], 32)
        P.op('dve', lambda e: e.tensor_scalar(out=gcol[0:4, 32:33], in0=gcol[0:4, 32:33], scalar1=-1.0, scalar2=0.0,
                                              op0=ALU.mult, op1=ALU.add), reads=[S_gcol], writes=[S_gcol])

        if l == 0:
            norm_T(lambda tt: D['x'][tt * 128:(tt + 1) * 128, :], lambda tt: [], D['mix_norm'][l], actT, S_act, 16,
                   copy=lambda tt: y_d[tt * 128:(tt + 1) * 128, :], S_copy=lambda tt: S_y[tt])
        else:
            norm_T(lambda tt: y_d[tt * 128:(tt + 1) * 128, :], lambda tt: S_y[tt], D['mix_norm'][l], actT, S_act, 16)
        if l == 0:
            dbg_out('d_xnT', actT, S_act)

        def qk_bufs():
            b = {}
            b['qT'] = alloc([128, 4, S], BF16)
            b['S_q'] = [P.slots(4) for _ in range(4)]
            b['kT'] = alloc([128, 4, S], BF16)
            b['S_k'] = [P.slots(4) for _ in range(4)]
            b['v'] = alloc([128, 16, 512], BF16)
            b['S_v'] = P.slots(16)
            b['oT32'] = alloc([128, 4, 512], F32)
            b['S_o'] = P.slots(4)
            b['ostage'] = alloc([128, 4, 512], BF16)
            b['S_ost'] = P.slot()
            return b

        def v_proj(b, wv, S_wv, c0):
            for t in range(16):
                bank, S_b = psA.next()
                gemm_tm(bank, S_b, 512, wv, S_wv, c0, t)
                evac(bank, S_b, b['v'][:, t, :], [b['S_v'][t]], eng='act' if t % 2 == 0 else 'dve')

        def qk_proj(b, key, Sk, w, S_w, gc):
            for tt in range(4):
                for h in range(4):
                    proj_norm_fm(w, S_w, h * 128, 128, tt, gc, b[key][:, h, tt * 512:(tt + 1) * 512], [b[Sk][h][tt]])
            pn_flush()

        @run_if('A')
        def ph_A():
            phase()
            wbufs_alloc(2)
            norm_bufs(0)
            pn_bufs()
            attn_bufs()
            b = qk_bufs()
            sp32 = alloc([4, 512], F32)
            cs = alloc([4, S], F32)
            S_sp = P.slot()
            S_cs = P.slot()
            ones4 = alloc([4, 512], F32)
            S_ones4 = P.slot()
            ckey = alloc([128, 64], F32)
            S_ckey = P.slot()
            dqbcs = [alloc([128, 512], F32) for _ in range(2)]
            S_dqs = P.slots(2)
            dqmap = {}
            etmp = alloc([4, 512], F32)
            S_et = P.slot()
            P.op('pool', lambda e: e.memset(ones4, 1.0), writes=[S_ones4])
            wq, S_wq = load_slab(w_in[:, OFF['fq']:OFF['fq'] + 512], 16, 512)
            wk, S_wk = load_slab(w_in[:, OFF['fk']:OFF['fk'] + 512], 16, 512)
            qk_proj(b, 'qT', 'S_q', wq, S_wq, 0)
            wv, S_wv = load_slab(w_in[:, OFF['fv']:OFF['fv'] + 516], 16, 516)
            qk_proj(b, 'kT', 'S_k', wk, S_wk, 1)
            v_proj(b, wv, S_wv, 0)
            for tt in range(4):
                bank, S_b = psA.next()
                gemm_fm(bank, S_b, 4, wv, S_wv, 512, tt)
                P.op('act', lambda e, bank=bank: e.activation(out=etmp, in_=bank[0:4, :], func=AF.Exp,
                                                              bias=gcol[0:4, 32:33], scale=-1.0),
                     reads=[S_b, S_gcol], writes=[S_et])
                P.op('act', lambda e, tt=tt: e.activation(out=sp32, in_=etmp, func=AF.Ln,
                                                          bias=1.0, scale=1.0), reads=[S_et], writes=[S_sp])
                init = 0.0 if tt == 0 else cs[:, tt * 512 - 1:tt * 512]
                P.op('dve', lambda e, tt=tt, init=init: e.tensor_tensor_scan(
                    out=cs[:, tt * 512:(tt + 1) * 512], data0=ones4, data1=sp32,
                    initial=init, op0=ALU.mult, op1=ALU.add), reads=[S_sp, S_ones4, S_cs], writes=[S_cs])
            bank, S_b = psA.next()
            for t in range(16):
                P.op('pe', lambda e, t=t, bank=bank: e.transpose(out=bank[:, t * 4:(t + 1) * 4],
                                                                 in_=cs[:, t * 128:(t + 1) * 128],
                                                                 identity=ident32[0:4, 0:4]),
                     reads=[S_cs, S_c], writes=[S_b])
            P.op('dve', lambda e, bank=bank: e.tensor_copy(out=ckey, in_=bank[:, 0:64]), reads=[S_b], writes=[S_ckey])
            dbg_out('d_cs', cs, [S_cs])
            dbg_out('d_fq', b['qT'], [s for hh in b['S_q'] for s in hh])

            def fox_setup(qt, h):
                di = len(dqmap) % 2
                dqmap[(qt, h)] = di
                dqbc, S_dq = dqbcs[di], S_dqs[di]
                bank, S_b = psM.next()
                P.op('pe', lambda e: e.matmul(bank, lhsT=selneg[:, h * 128:(h + 1) * 128],
                                              rhs=cs[:, qt * 512:(qt + 1) * 512], start=True, stop=True),
                     reads=[S_cs, S_c], writes=[S_b])
                P.op('act', lambda e: e.activation(out=dqbc, in_=bank, func=AF.Copy), reads=[S_b], writes=[S_dq])

            def fox_dve_bias(qt, h, kt, sb, S_sb, c0):
                di = dqmap[(qt, h)]
                dqbc, S_dq = dqbcs[di], S_dqs[di]
                P.op('dve', lambda e: e.tensor_tensor(out=sb[:, c0:512], in0=sb[:, c0:512], in1=dqbc[:, c0:512],
                                                      op=ALU.add), reads=[S_sb, S_dq], writes=[S_sb])

            attention(range(4), 4,
                      lambda h, kt, qa, qb_, c0: [(b['kT'][:, h, kt * 128:(kt + 1) * 128], b['qT'][:, h, qa:qb_],
                                                  [b['S_k'][h][kt // 4], b['S_q'][h][qa // 512]])],
                      lambda h, kt: (b['v'][:, kt, h * 128:(h + 1) * 128], [b['S_v'][kt]]),
                      128 ** -0.5, True, 16, b['oT32'], b['S_o'],
                      dve_bias=fox_dve_bias,
                      act_bias=lambda h, kt: (ckey[:, kt * 4 + h:kt * 4 + h + 1], [S_ckey]),
                      setup=fox_setup, tail=group_tail(0, b['oT32'], b['S_o'], b['ostage'], b['S_ost']))

        @run_if('B')
        def ph_B():
            phase()
            b = {}
            b['qT'] = alloc([128, 4, S], BF16)
            b['S_q'] = [P.slots(4) for _ in range(4)]
            b['kT'] = alloc([128, 4, S], BF16)
            b['S_k'] = [P.slots(4) for _ in range(4)]
            b['v'] = alloc([128, 16, 512], BF16)
            b['S_v'] = P.slots(16)
            qpe = alloc([64, 4, S], BF16)
            S_qpe = [P.slots(4) for _ in range(4)]
            kpe = alloc([64, S], BF16)
            S_kpe = P.slots(4)
            keepB = st['off']
            wbufs_alloc(2, [512, 320])
            norm_bufs(4)
            pn_bufs()
            cqn = alloc([128, 4, 512], BF16)
            S_cqn = P.slot()
            ckvn = alloc([128, 2, 512], BF16)
            S_ckvn = P.slot()
            cos2 = alloc([64, 512], F32)
            sin2 = alloc([64, 512], F32)
            S_cs2 = P.slot()
            x32 = nb['tmp'][0:64, 1, :]
            S_x32 = nb['S_tmp'][1]
            t1 = nb['tmp'][0:64, 2, :]
            S_t1 = nb['S_tmp'][2]
            t2 = nb['tmp'][0:64, 3, :]
            S_t2 = nb['S_tmp'][3]
            wuq = alloc([128, 4, 768], BF16)
            wukv = alloc([128, 2, 1024], BF16)
            S_wu = P.slot()
            P.dma('pool', lambda e: e.dma_start(out=wuq, in_=D['mla_w_uq'][l].rearrange("(kc p) n -> p kc n", p=128)),
                  writes=[S_wu])
            P.dma('pool', lambda e: e.dma_start(out=wukv, in_=D['mla_w_ukv'][l].rearrange("(kc p) n -> p kc n", p=128)),
                  writes=[S_wu])
            wa, S_wa = load_slab(w_in[:, OFF['mcq']:OFF['mcq'] + 512], 16, 512)
            wb, S_wb = load_slab(w_in[:, OFF['mckv']:OFF['mckv'] + 320], 16, 320)

            def rope(src32, S_src, out, S_out):
                bank, S_b = psB.next()
                P.op('pe', lambda e: e.matmul(bank[0:64, :], lhsT=rotT, rhs=src32, start=True, stop=True),
                     reads=[S_src, S_c], writes=[S_b])
                P.op('dve', lambda e: e.tensor_tensor(out=t1, in0=src32, in1=cos2, op=ALU.mult),
                     reads=[S_src, S_cs2], writes=[S_t1])
                P.op('dve', lambda e: e.tensor_tensor(out=t2, in0=bank[0:64, :], in1=sin2, op=ALU.mult),
                     reads=[S_b, S_cs2], writes=[S_t2])
                P.op('dve', lambda e: e.tensor_tensor(out=out, in0=t1, in1=t2, op=ALU.add),
                     reads=[S_t1, S_t2], writes=S_out)

            for tt in range(4):
                P.dma('sp', lambda e, tt=tt: e.dma_start(out=cos2, in_=cs_d[0:64, tt * 512:(tt + 1) * 512]), writes=[S_cs2])
                P.dma('sp', lambda e, tt=tt: e.dma_start(out=sin2, in_=cs_d[64:128, tt * 512:(tt + 1) * 512]), writes=[S_cs2])
                for c in range(4):
                    bank, S_b = psA.next()
                    gemm_fm(bank, S_b, 128, wa, S_wa, c * 128, tt)
                    evac(bank, S_b, nb['tmp'][:, c, :], [nb['S_tmp'][c]], eng='act' if c % 2 == 0 else 'dve')
                fm_norm(nb['tmp'], nb['S_tmp'], 128, 4, 2, lambda c: cqn[:, c, :], lambda c: [S_cqn])
                for c in range(2):
                    bank, S_b = psA.next()
                    gemm_fm(bank, S_b, 128, wb, S_wb, c * 128, tt)
                    evac(bank, S_b, nb['tmp'][:, c, :], [nb['S_tmp'][c]], eng='act' if c % 2 == 0 else 'dve')
                fm_norm(nb['tmp'], nb['S_tmp'], 128, 2, 6, lambda c: ckvn[:, c, :], lambda c: [S_ckvn])
                bank, S_b = psA.next()
                gemm_fm(bank, S_b, 64, wb, S_wb, 256, tt)
                evac(bank, S_b, nb['tmp'][0:64, 0, :], [nb['S_tmp'][0]], M=64)
                fm_norm(nb['tmp'], nb['S_tmp'], 64, 1, 11, lambda c: x32, lambda c: [S_x32])
                rope(x32, S_x32, kpe[:, tt * 512:(tt + 1) * 512], [S_kpe[tt]])
                for h in range(4):
                    proj_norm_fm(wuq, S_wu, h * 192, 128, tt, 8, b['qT'][:, h, tt * 512:(tt + 1) * 512],
                                 [b['S_q'][h][tt]], src=cqn, S_src=[S_cqn], nk=4, t0=0)
                    proj_norm_fm(wuq, S_wu, h * 192 + 128, 64, tt, 9, x32, [S_x32], src=cqn, S_src=[S_cqn], nk=4, t0=0,
                                 post=(lambda h=h, tt=tt: rope(x32, S_x32, qpe[:, h, tt * 512:(tt + 1) * 512],
                                                               [S_qpe[h][tt]])))
                    proj_norm_fm(wukv, S_wu, h * 256, 128, tt, 10, b['kT'][:, h, tt * 512:(tt + 1) * 512],
                                 [b['S_k'][h][tt]], src=ckvn, S_src=[S_ckvn], nk=2, t0=0)
                pn_flush()
                for j in range(4):
                    t = tt * 4 + j
                    bank, S_b = psA.next()
                    for h in range(4):
                        for kc in range(2):
                            P.op('pe', lambda e, bank=bank, h=h, kc=kc, j=j: e.matmul(
                                bank[:, h * 128:(h + 1) * 128], lhsT=ckvn[:, kc, j * 128:(j + 1) * 128],
                                rhs=wukv[:, kc, h * 256 + 128:h * 256 + 256], start=(kc == 0), stop=(kc == 1)),
                                reads=[S_ckvn, S_wu], writes=[S_b])
                    evac(bank, S_b, b['v'][:, t, :], [b['S_v'][t]], eng='dve')
            dbg_out('d_mq', b['qT'], [s for hh in b['S_q'] for s in hh])
            dbg_out('d_mqpe', qpe, [s for hh in S_qpe for s in hh])
            dbg_out('d_mkpe', kpe, S_kpe)
            P.barrier()
            st['off'] = keepB
            norm_bufs(0)
            attn_bufs()
            b['oT32'] = alloc([128, 4, 512], F32)
            b['S_o'] = P.slots(4)
            b['ostage'] = alloc([128, 4, 512], BF16)
            b['S_ost'] = P.slot()

            attention(range(4), 4,
                      lambda h, kt, qa, qb_, c0: [
                          (b['kT'][:, h, kt * 128:(kt + 1) * 128], b['qT'][:, h, qa:qb_],
                           [b['S_k'][h][kt // 4], b['S_q'][h][qa // 512]]),
                          (kpe[:, kt * 128:(kt + 1) * 128], qpe[:, h, qa:qb_], [S_kpe[kt // 4], S_qpe[h][qa // 512]])],
                      lambda h, kt: (b['v'][:, kt, h * 128:(h + 1) * 128], [b['S_v'][kt]]),
                      192 ** -0.5, True, 16, b['oT32'], b['S_o'],
                      tail=group_tail(1, b['oT32'], b['S_o'], b['ostage'], b['S_ost']))

        @run_if('C')
        def ph_C():
            phase()
            wbufs_alloc(2)
            norm_bufs(0)
            uT = alloc([128, 4, S], BF16)
            S_u = [P.slots(4) for _ in range(4)]
            vln = alloc([128, 16, 512], BF16)
            S_vln = P.slots(16)
            g1s = [alloc([128, 512], F32) for _ in range(2)]
            g2s = [alloc([128, 512], F32) for _ in range(2)]
            S_g1s = P.slots(2)
            S_g2s = P.slots(2)
            vgs = [alloc([128, 512], F32) for _ in range(2)]
            S_vgs = P.slots(2)
            grot = Rot([0, 1])
            lnbc = alloc([128, 1024], F32)
            bsbc = alloc([128, 512], F32)
            S_ln = P.slot()
            stats = [alloc([128, 8], F32) for _ in range(2)]
            S_stats = P.slots(2)
            ws32 = alloc([128, 4, 128], F32)
            S_ws = P.slot()
            wcT = alloc([128, 4, 128], BF16)
            S_wcT = P.slot()
            oT32 = alloc([128, 4, 512], F32)
            S_o = P.slots(4)
            ostage = alloc([128, 4, 512], BF16)
            S_ost = P.slot()
            otmp = alloc([128, 4, 128], F32)
            S_otmp = P.slots(4)
            junks = [alloc([128, 512], F32) for _ in range(2)]
            S_junks = P.slots(2)
            P.dma('sp', lambda e: e.dma_start(out=lnbc[:, 0:512], in_=D['gmlp_ln_g'][l].partition_broadcast(128)), writes=[S_ln])
            P.dma('sp', lambda e: e.dma_start(out=lnbc[:, 512:1024], in_=D['gmlp_ln_b'][l].partition_broadcast(128)), writes=[S_ln])
            P.dma('sp', lambda e: e.dma_start(out=bsbc, in_=D['gmlp_b_s'][l].rearrange("g t -> (g t)").partition_broadcast(128)),
                  writes=[S_ln])
            P.dma('sp', lambda e: e.dma_start(out=ws32, in_=D['gmlp_w_s'][l].rearrange("g t s -> t g s")), writes=[S_ws])
            for g in range(4):
                P.op('dve', lambda e, g=g: e.tensor_tensor(out=ws32[:, g, :], in0=ws32[:, g, :], in1=tril, op=ALU.mult),
                     reads=[S_ws, S_c], writes=[S_ws])
            bank, S_b = psA.next()
            for g in range(4):
                P.op('pe', lambda e, g=g, bank=bank: e.transpose(out=bank[:, g * 128:(g + 1) * 128], in_=ws32[:, g, :],
                                                                 identity=ident32), reads=[S_ws, S_c], writes=[S_b])
            P.op('dve', lambda e, bank=bank: e.tensor_copy(out=wcT, in_=bank.rearrange("p (g t) -> p g t", g=4)),
                 reads=[S_b], writes=[S_wcT])

            def gelu(src, S_src, out, S_out):
                gi_ = grot.next()
                g1, g2, S_g1, S_g2 = g1s[gi_], g2s[gi_], S_g1s[gi_], S_g2s[gi_]
                P.op('act', lambda e: e.activation(out=g1, in_=src, func=AF.Square), reads=[S_src], writes=[S_g1])
                P.op('dve', lambda e: e.tensor_scalar(out=g1, in0=g1, scalar1=0.044715, scalar2=1.0, op0=ALU.mult,
                                                      op1=ALU.add), reads=[S_g1], writes=[S_g1])
                P.op('dve', lambda e: e.tensor_tensor(out=g1, in0=src, in1=g1, op=ALU.mult), reads=[S_src, S_g1],
                     writes=[S_g1])
                P.op('act', lambda e: e.activation(out=g2, in_=g1, func=AF.Sigmoid, scale=1.5957691216057308),
                     reads=[S_g1], writes=[S_g2])
                P.op('dve', lambda e: e.tensor_tensor(out=out, in0=src, in1=g2, op=ALU.mult), reads=[S_src, S_g2],
                     writes=S_out)

            wu_, S_wu_ = load_slab(w_in[:, OFF['gu']:OFF['gu'] + 512], 16, 512)
            wv_, S_wv_ = load_slab(w_in[:, OFF['gv']:OFF['gv'] + 512], 16, 512)
            for tt in range(4):
                for c in range(4):
                    bank, S_b = psA.next()
                    gemm_fm(bank, S_b, 128, wu_, S_wu_, c * 128, tt)
                    gelu(bank, S_b, uT[:, c, tt * 512:(tt + 1) * 512], [S_u[c][tt]])
            def vln_part1(t, vg, S_vg):
                bank, S_b = psA.next()
                gemm_tm(bank, S_b, 512, wv_, S_wv_, 0, t)
                gelu(bank, S_b, vg, [S_vg])

            def vln_stage(t, vg, S_vg, stat, S_stat, junk, S_junk):
                P.op('pool', lambda e: e.memset(stat, 0.0), writes=[S_stat])
                P.op('act', lambda e: e.activation(out=junk, in_=vg, func=AF.Identity, accum_out=stat[:, 0:1]),
                     reads=[S_vg], writes=[S_junk, S_stat])
                P.op('act', lambda e: e.activation(out=junk, in_=vg, func=AF.Square, accum_out=stat[:, 1:2]),
                     reads=[S_vg], writes=[S_junk, S_stat])
                P.op('dve', lambda e: e.tensor_scalar(out=stat[:, 2:3], in0=stat[:, 0:1], scalar1=1.0 / 512, scalar2=0.0,
                                                      op0=ALU.mult, op1=ALU.add), reads=[S_stat], writes=[S_stat])
                P.op('dve', lambda e: e.tensor_tensor(out=stat[:, 3:4], in0=stat[:, 2:3], in1=stat[:, 2:3], op=ALU.mult),
                     reads=[S_stat], writes=[S_stat])
                P.op('dve', lambda e: e.scalar_tensor_tensor(out=stat[:, 4:5], in0=stat[:, 1:2], scalar=1.0 / 512,
                                                             in1=stat[:, 3:4], op0=ALU.mult, op1=ALU.subtract),
                     reads=[S_stat], writes=[S_stat])
                P.op('act', lambda e: e.activation(out=stat[:, 5:6], in_=stat[:, 4:5], func=AF.Sqrt, bias=EPS, scale=1.0),
                     reads=[S_stat], writes=[S_stat])
                P.op('dve', lambda e: e.reciprocal(out=stat[:, 5:6], in_=stat[:, 5:6]), reads=[S_stat], writes=[S_stat])
                P.op('dve', lambda e: e.tensor_scalar(out=vg, in0=vg, scalar1=stat[:, 2:3], scalar2=stat[:, 5:6],
                                                      op0=ALU.subtract, op1=ALU.mult), reads=[S_vg, S_stat], writes=[S_vg])
                P.op('pool', lambda e: e.tensor_tensor(out=vg, in0=vg, in1=lnbc[:, 0:512], op=ALU.mult),
                     reads=[S_vg, S_ln], writes=[S_vg])
                P.op('pool', lambda e, t=t: e.tensor_tensor(out=vln[:, t, :], in0=vg, in1=lnbc[:, 512:1024], op=ALU.add),
                     reads=[S_vg, S_ln], writes=[S_vln[t]])
            vln_part1(0, vgs[0], S_vgs[0])
            for t in range(16):
                if t + 1 < 16:
                    vln_part1(t + 1, vgs[(t + 1) % 2], S_vgs[(t + 1) % 2])
                vg, S_vg, stat, S_stat, junk, S_junk = vgs[t % 2], S_vgs[t % 2], stats[t % 2], S_stats[t % 2], junks[t % 2], S_junks[t % 2]
                vln_stage(t, vg, S_vg, stat, S_stat, junk, S_junk)

            for qt in range(4):
                for g in range(4):
                    bank, S_b = psA.next()
                    for j in range(4):
                        t = qt * 4 + j
                        P.op('pe', lambda e, bank=bank, j=j, t=t, g=g: e.matmul(
                            bank[:, j * 128:(j + 1) * 128], lhsT=vln[:, t, g * 128:(g + 1) * 128], rhs=wcT[:, g, :],
                            start=True, stop=True), reads=[S_vln[t], S_wcT], writes=[S_b])
                    for j in range(4):
                        P.op('dve', lambda e, bank=bank, j=j, g=g: e.tensor_tensor(
                            out=otmp[:, j, :], in0=bank[:, j * 128:(j + 1) * 128], in1=bsbc[:, g * 128:(g + 1) * 128], op=ALU.add),
                            reads=[S_b, S_ln], writes=[S_otmp[j]])
                    P.op('dve', lambda e, g=g, qt=qt: e.tensor_tensor(
                        out=oT32[:, g, :], in0=otmp.rearrange("p a b -> p (a b)"),
                        in1=uT[:, g, qt * 512:(qt + 1) * 512], op=ALU.mult),
                        reads=S_otmp + [S_u[g][qt]], writes=[S_o[g]])
                group_tail(2, oT32, S_o, ostage, S_ost)(qt)

        @run_if('D')
        def ph_D():
            phase()
            wbufs_alloc(2)
            norm_bufs(0)
            pn_bufs()
            attn_bufs()
            b = qk_bufs()
            km32 = alloc([128, 8], F32)
            S_km32 = P.slot()
            kmean = alloc([128, 4, 8], BF16)
            S_km = P.slot()
            g8 = alloc([128, 128], F32)
            m8 = alloc([128, 128], F32)
            b8 = alloc([128, 128], F32)
            S_g8 = P.slot()
            S_m8 = P.slots(16)
            S_b8 = P.slots(16)
            biasT = alloc([8, 4, S], BF16)
            S_bT = [P.slots(4) for _ in range(4)]
            wq, S_wq = load_slab(w_in[:, OFF['bq']:OFF['bq'] + 512], 16, 512)
            wk, S_wk = load_slab(w_in[:, OFF['bk']:OFF['bk'] + 512], 16, 512)
            qk_proj(b, 'qT', 'S_q', wq, S_wq, 12)
            wv, S_wv = load_slab(w_in[:, OFF['bv']:OFF['bv'] + 512], 16, 512)
            qk_proj(b, 'kT', 'S_k', wk, S_wk, 13)
            v_proj(b, wv, S_wv, 0)
            for h in range(4):
                P.op('dve', lambda e, h=h: e.tensor_reduce(out=km32, in_=b['kT'][:, h, :].rearrange("p (n k) -> p n k", k=256),
                                                           axis=AX.X, op=ALU.add), reads=b['S_k'][h], writes=[S_km32])
                P.op('dve', lambda e, h=h: e.tensor_scalar(out=kmean[:, h, :], in0=km32, scalar1=1.0 / 256, scalar2=0.0,
                                                           op0=ALU.mult, op1=ALU.add), reads=[S_km32], writes=[S_km])

            for h in range(4):
                gb, S_gb = psA.next()
                for t in range(16):
                    P.op('pe', lambda e, gb=gb, t=t, h=h: e.matmul(gb[:, t * 8:(t + 1) * 8],
                                                                   lhsT=b['qT'][:, h, t * 128:(t + 1) * 128],
                                                                   rhs=kmean[:, h, :], start=True, stop=True),
                         reads=[b['S_q'][h][t // 4], S_km], writes=[S_gb])
                P.op('dve', lambda e, gb=gb: e.tensor_tensor(out=g8, in0=gb[:, 0:128], in1=mneg16, op=ALU.add),
                     reads=[S_gb, S_c], writes=[S_g8])
                for t in range(16):
                    P.op('dve', lambda e, t=t: e.max(out=m8[:, t * 8:(t + 1) * 8], in_=g8[:, t * 8:(t + 1) * 8]),
                         reads=[S_g8], writes=[S_m8[t]])
                    P.op('dve', lambda e, t=t: e.tensor_scalar(out=b8[:, t * 8:(t + 1) * 8], in0=g8[:, t * 8:(t + 1) * 8],
                                                               scalar1=m8[:, t * 8 + 3:t * 8 + 4], scalar2=1.0,
                                                               op0=ALU.is_ge, op1=ALU.subtract),
                         reads=[S_g8, S_m8[t]], writes=[S_b8[t]])
                for q4 in range(4):
                    tb, S_tb = psB.next()
                    for j in range(4):
                        t = q4 * 4 + j
                        P.op('pe', lambda e, tb=tb, j=j, t=t: e.transpose(out=tb[0:8, j * 128:(j + 1) * 128],
                                                                          in_=b8[:, t * 8:(t + 1) * 8], identity=ident32),
                             reads=[S_b8[t], S_c], writes=[S_tb])
                    P.op('act', lambda e, tb=tb, h=h, q4=q4: e.activation(out=biasT[:, h, q4 * 512:(q4 + 1) * 512],
                                                                          in_=tb[0:8, :], func=AF.Copy, scale=-NEGBIG),
                         reads=[S_tb], writes=[S_bT[h][q4]])

            attention(range(4), 4,
                      lambda h, kt, qa, qb_, c0: [
                          (b['kT'][:, h, kt * 128:(kt + 1) * 128], b['qT'][:, h, qa:qb_],
                           [b['S_k'][h][kt // 4], b['S_q'][h][qa // 512]]),
                          (onehot8[:, (kt // 2) * 128:(kt // 2 + 1) * 128], biasT[:, h, qa:qb_], [S_c, S_bT[h][qa // 512]])],
                      lambda h, kt: (b['v'][:, kt, h * 128:(h + 1) * 128], [b['S_v'][kt]]),
                      128 ** -0.5, True, 16, b['oT32'], b['S_o'],
                      tail=group_tail(3, b['oT32'], b['S_o'], b['ostage'], b['S_ost']))

        def accum_tile(t, gemm_for_fb, yst, S_yst, rot, fbs=(0, 1, 2, 3)):
            i = rot.next()
            for fb in fbs:
                bank, S_b = psA.next()
                gemm_for_fb(fb, bank, S_b)
                evac(bank, S_b, yst[i][:, fb * 512:(fb + 1) * 512], [S_yst[i][fb]], eng='act' if fb % 2 == 0 else 'dve')
            ca, cb_ = fbs[0] * 512, (fbs[-1] + 1) * 512
            P.dma('pool', lambda e: e.dma_start(out=y_d[t * 128:(t + 1) * 128, ca:cb_], in_=yst[i][:, ca:cb_],
                                                accum_op=ALU.add),
                  reads=[S_yst[i][fb] for fb in fbs], writes=[S_y[t][fb] for fb in fbs])

        @run_if('O')
        def ph_O():
            phase()
            wbufs_alloc(4)
            yst = [alloc([128, DM], F32) for _ in range(3)]
            S_yst = [P.slots(4) for _ in range(3)]
            yrot = Rot([0, 1, 2])
            for g in range(4):
                P.dma('sp', lambda e, g=g: e.dma_start(out=actT[:, g * 4:(g + 1) * 4, :],
                                                       in_=mixT_d[g * 512:(g + 1) * 512, :].rearrange("(c p) t -> p c t", p=128)),
                      reads=S_mix[g], writes=S_act)
            dbg_out('d_mixT', actT, S_act)
            wos = [load_slab(D['w_out'][l][:, fb * 512:(fb + 1) * 512], 16, 512) for fb in range(4)]
            nctx = norm_prep(D['xattn_norm'][l])
            ysrc = lambda tt: y_d[tt * 128:(tt + 1) * 128, :]
            for fbs in ((0, 1), (2, 3)):
                for t in range(16):
                    accum_tile(t, lambda fb, bank, S_b, t=t: gemm_tm(bank, S_b, 512, wos[fb][0], wos[fb][1], 0, t),
                               yst, S_yst, yrot, fbs=fbs)
                    if fbs[0] == 2 and t >= 2:
                        norm_tile(nctx, t - 2, ysrc, lambda tt: S_y[tt], actT, S_act, banks=psB)
            for t in (14, 15):
                norm_tile(nctx, t, ysrc, lambda tt: S_y[tt], actT, S_act, banks=psB)

        @run_if('X')
        def ph_X():
            phase()
            memT = alloc([128, 16, 256], BF16)
            S_memT = P.slots(2)
            keep = st['off']
            norm_T(lambda tt: D['mem'][tt * 128:(tt + 1) * 128, :], lambda tt: [], D['mem_norm'][l], memT, S_memT, 2)
            P.barrier()
            st['off'] = keep
            wbufs_alloc(2)
            norm_bufs(1)
            pn_bufs()
            attn_bufs()
            qT = alloc([128, 4, S], BF16)
            S_q = [P.slots(4) for _ in range(4)]
            xkT = alloc([128, 4, 256], BF16)
            S_xk = P.slots(4)
            xv = alloc([128, 2, 512], BF16)
            S_xv = P.slots(2)
            oT32 = alloc([128, 4, 512], F32)
            S_o = P.slots(4)
            aT = alloc([128, 4, S], BF16)
            S_a = P.slots(4)
            yst = [alloc([128, DM], F32) for _ in range(2)]
            S_yst = [P.slots(4) for _ in range(2)]
            yrot = Rot([0, 1])
            wk, S_wk = load_slab(D['w_xkv'][l][:, 0:512], 16, 512)
            wv, S_wv = load_slab(D['w_xkv'][l][:, 512:1024], 16, 512)
            for h in range(4):
                bank, S_b = psA.next()
                gemm_fm(bank, S_b, 128, wk, S_wk, h * 128, 0, width=256, src=memT, S_src=S_memT, t0=0)
                evac(bank, S_b, nb['tmp'][:, 0, 0:256], [nb['S_tmp'][0]], width=256)
                fm_norm(nb['tmp'], nb['S_tmp'], 128, 1, 31, lambda c: xkT[:, h, :], lambda c: [S_xk[h]], width=256)
            for mt in range(2):
                bank, S_b = psA.next()
                gemm_tm(bank, S_b, 512, wv, S_wv, 0, mt, src=memT, S_src=[S_memT[mt]])
                evac(bank, S_b, xv[:, mt, :], [S_xv[mt]], eng='dve')
            wq, S_wq = load_slab(D['w_xq'][l], 16, 512)
            for tt in range(4):
                for h in range(4):
                    proj_norm_fm(wq, S_wq, h * 128, 128, tt, 30, qT[:, h, tt * 512:(tt + 1) * 512], [S_q[h][tt]])
            pn_flush()

            def x_tail(qt):
                P.op('act', lambda e: e.activation(out=aT[:, :, qt * 512:(qt + 1) * 512], in_=oT32, func=AF.Copy),
                     reads=S_o, writes=[S_a[qt]])

            attention(range(4), 4,
                      lambda h, kt, qa, qb_, c0: [(xkT[:, h, kt * 128:(kt + 1) * 128], qT[:, h, qa:qb_],
                                                  [S_xk[h], S_q[h][qa // 512]])],
                      lambda h, kt: (xv[:, kt, h * 128:(h + 1) * 128], [S_xv[kt]]),
                      128 ** -0.5, False, 2, oT32, S_o, tail=x_tail)
            wo, S_wo = load_slab(D['w_xo'][l], 4, 2048, kind='rows')
            for t in range(16):
                accum_tile(t, lambda fb, bank, S_b, t=t: gemm_tm(bank, S_b, 512, wo, S_wo, fb * 512, t, src=aT,
                                                                 S_src=[S_a[t // 4]], nk=4), yst, S_yst, yrot)

        @run_if('F')
        def ph_F():
            phase()
            norm_T(lambda tt: y_d[tt * 128:(tt + 1) * 128, :], lambda tt: S_y[tt], D['ffn_norm'][l], actT, S_act, 16)
            phase()
            wbufs_alloc(5)
            hT = alloc([128, 8, S], BF16)
            S_h = [P.slots(4) for _ in range(8)]
            sg = [alloc([128, 512], F32) for _ in range(2)]
            S_sg = P.slots(2)
            yst = [alloc([128, DM], F32) for _ in range(2)]
            S_yst = [P.slots(4) for _ in range(2)]
            yrot = Rot([0, 1])
            srot = Rot([0, 1])
            wgu = D['w_gate_up'][l]
            wdn = D['w_down'][l]
            NCH = DFF // 128
            groups = [(c0, min(8, NCH - c0)) for c0 in range(0, NCH, 8)]
            specs = []
            for (c0, n) in groups:
                for hf in range(n // 4):
                    cc = (c0 + 4 * hf) * 128
                    specs.append((wgu[:, cc:cc + 512], 16, 512, 'k16'))
                    specs.append((wgu[:, DFF + cc:DFF + cc + 512], 16, 512, 'k16'))
                for hf in range(n // 4):
                    cc = (c0 + 4 * hf) * 128
                    specs.append((wdn[cc:cc + 512, :], 4, 2048, 'rows'))
            loaded = []

            def get(j):
                while len(loaded) < min(len(specs), j + 5):
                    loaded.append(load_slab(*specs[len(loaded)]))
                return loaded[j]

            j = 0
            for (c0, n) in groups:
                nh = n // 4
                for hf in range(nh):
                    wg, S_wg = get(j)
                    wu_, S_wu_ = loaded[j + 1]
                    j += 2
                    for c4 in range(4):
                        c = hf * 4 + c4
                        for tt in range(4):
                            bg, S_bg = psA.next()
                            gemm_fm(bg, S_bg, 128, wg, S_wg, c4 * 128, tt)
                            bu, S_bu = psB.next()
                            gemm_fm(bu, S_bu, 128, wu_, S_wu_, c4 * 128, tt)
                            si = srot.next()
                            P.op('act', lambda e, bg=bg, si=si: e.activation(out=sg[si], in_=bg, func=AF.Silu),
                                 reads=[S_bg], writes=[S_sg[si]])
                            P.op('dve', lambda e, bu=bu, si=si, c=c, tt=tt: e.tensor_tensor(
                                out=hT[:, c, tt * 512:(tt + 1) * 512], in0=bu, in1=sg[si], op=ALU.mult),
                                reads=[S_bu, S_sg[si]], writes=[S_h[c][tt]])
                get(j)
                wds = [loaded[j + hf] for hf in range(nh)]
                j += nh
                for t in range(16):
                    def down(fb, bank, S_b, t=t, wds=wds, nh=nh):
                        nk = 4 * nh
                        for kc in range(nk):
                            wd, S_wd = wds[kc // 4]
                            P.op('pe', lambda e, kc=kc, wd=wd: e.matmul(
                                bank[:, 0:512], lhsT=hT[:, kc, t * 128:(t + 1) * 128],
                                rhs=wd[:, kc % 4, fb * 512:(fb + 1) * 512], start=(kc == 0), stop=(kc == nk - 1)),
                                reads=[S_wd, S_h[kc][t // 4]], writes=[S_b])
                    accum_tile(t, down, yst, S_yst, yrot)

    for l in range(L):
        layer(l)
    P.emit()
    es.close()
    return P


_CONSTS = None


def kernel(**inputs):
    global _CONSTS
    nc = bass.Bass("TRN2", target_bir_lowering=False)
    build(nc, L=2)
    if _CONSTS is None:
        _CONSTS = host_consts()
    shared = {}
    for name, shape in WSH:
        shared[name] = np.ascontiguousarray(np.asarray(inputs[name], dtype=np.float32))
    shared.update(_CONSTS)
    x = np.asarray(inputs['x'], dtype=np.float32)
    mem = np.asarray(inputs['mem'], dtype=np.float32)
    in_maps = []
    for b in range(8):
        m = dict(shared)
        m['x'] = np.ascontiguousarray(x[b])
        m['mem'] = np.ascontiguousarray(mem[b])
        in_maps.append(m)
    res = run_bass_kernel_spmd(nc, in_maps, core_ids=list(range(8)))
    return np.stack([np.asarray(r['y'], dtype=np.float32) for r in res.results], axis=0)
```
